# Optimizing a Trainium2 kernel written in Bass

```python
import math, functools
import jax, jax.numpy as jnp
from jax import lax
import numpy as np

D_MODEL = 2048
BATCH = 2
SEQ = 4096
DEPTH = 1
DEC_BATCH = 32
DEC_SEQ = 8
PAST_LEN = 8192
PAGE_SIZE = 128

D_MIX = D_MODEL
D_MLSTM = D_MIX // 2
N_MLSTM_HEADS = 4
HD_MLSTM = D_MLSTM // N_MLSTM_HEADS
D_ATT = D_MIX - D_MLSTM
N_ATT_HEADS = 8
HD_ATT = D_ATT // N_ATT_HEADS
DILATED_CONFIGS = ((128, 1), (512, 4), (2048, 16))
MAX_WINDOW = max(w for w, _ in DILATED_CONFIGS)
BAND_BLOCK = 128
MLSTM_CHUNK = 64
CONV_W = 4
N_GROUPS = 4
E_PER_GROUP = 8
N_EXPERTS = N_GROUPS * E_PER_GROUP
TOP_K = 2
D_FF_EXPERT = D_MODEL // 2
ALPHA = (2.0 * DEPTH) ** 0.25
BETA = (8.0 * DEPTH) ** -0.25
EPS = 1e-5
IN_SPLITS = (2 * D_MLSTM, 3 * D_MLSTM, 4 * D_MLSTM, 4 * D_MLSTM + 2 * N_MLSTM_HEADS)
N_IN_COLS = 4 * D_MLSTM + 2 * N_MLSTM_HEADS + 3 * D_ATT

kernel_name = 'hymba_mlstm_dilated_hmoe_step'


def layer_norm(x, g, b):
    xf = x.astype(jnp.float32)
    mu = xf.mean(axis=-1, keepdims=True)
    var = jnp.mean(jnp.square(xf - mu), axis=-1, keepdims=True)
    return ((xf - mu) * lax.rsqrt(var + EPS) * g + b).astype(x.dtype)


def alibi_slopes():
    return jnp.asarray([2.0 ** (-8.0 * (h + 1) / N_ATT_HEADS) for h in range(N_ATT_HEADS)], dtype=jnp.float32)


def in_projection(x, w_in):
    B, T, _ = x.shape
    xp = jnp.einsum('btd,dc->btc', x, w_in)
    qk_raw, v_m, o_m, gates, qkv_a = jnp.split(xp, IN_SPLITS, axis=-1)
    v_m = v_m.reshape(B, T, N_MLSTM_HEADS, HD_MLSTM)
    qkv_a = qkv_a.reshape(B, T, 3, N_ATT_HEADS, HD_ATT)
    return qk_raw, v_m, o_m, gates, qkv_a[:, :, 0], qkv_a[:, :, 1], qkv_a[:, :, 2]


def mlstm_prep(u, gates, conv_w, conv_b, b_gate):
    B = u.shape[0]
    T = u.shape[1] - (CONV_W - 1)
    conv = conv_b
    for j in range(CONV_W):
        conv = conv + u[:, j:j + T] * conv_w[j]
    qk = jax.nn.silu(conv.astype(jnp.float32)).reshape(B, T, 2, N_MLSTM_HEADS, HD_MLSTM)
    q = qk[:, :, 0]
    k = qk[:, :, 1] * (HD_MLSTM ** -0.5)
    g = gates.astype(jnp.float32) + b_gate.astype(jnp.float32)
    ig = g[..., :N_MLSTM_HEADS]
    lf = jax.nn.log_sigmoid(g[..., N_MLSTM_HEADS:])
    return q, k, ig, lf


def mlstm_chunk(carry, inp):
    C, n, m = (a.astype(jnp.float32) for a in carry)
    q, k, v, ig, lf = inp
    L = q.shape[1]
    b = jnp.cumsum(lf, axis=1)
    a = b + m[:, None, :]
    causal = jnp.tril(jnp.ones((L, L), dtype=bool))
    dlog = jnp.where(causal[None, :, :, None],
                     b[:, :, None, :] - b[:, None, :, :] + ig[:, None, :, :], -jnp.inf)
    m_t = jnp.maximum(a, dlog.max(axis=2))
    w_intra = jnp.exp(dlog - m_t[:, :, None, :])
    w_inter = jnp.exp(a - m_t)
    s = jnp.einsum('bthe,bshe->btsh', q, k) * w_intra
    num = jnp.einsum('btsh,bshf->bthf', s, v) + w_inter[..., None] * jnp.einsum('bthe,bhef->bthf', q, C)
    den = s.sum(axis=2) + w_inter * jnp.einsum('bthe,bhe->bth', q, n)
    h = num / jnp.maximum(jnp.abs(den), jnp.exp(-m_t))[..., None]
    b_last = b[:, -1]
    log_w = b_last[:, None, :] - b + ig
    m_new = jnp.maximum(b_last + m, log_w.max(axis=1))
    w_upd = jnp.exp(log_w - m_new[:, None, :])
    decay = jnp.exp(b_last + m - m_new)
    C_new = decay[..., None, None] * C + jnp.einsum('bsh,bshe,bshf->bhef', w_upd, k, v)
    n_new = decay[..., None] * n + jnp.einsum('bsh,bshe->bhe', w_upd, k)
    return (C_new, n_new, m_new), h


def mlstm_prompt(q, k, v, ig, lf):
    B, T, H, E = q.shape
    n_chunks = T // MLSTM_CHUNK
    def to_chunks(a):
        return jnp.moveaxis(a.reshape(B, n_chunks, MLSTM_CHUNK, *a.shape[2:]), 1, 0)
    init = (jnp.zeros((B, H, E, E), jnp.float32), jnp.zeros((B, H, E), jnp.float32), jnp.zeros((B, H), jnp.float32))
    state, h = lax.scan(mlstm_chunk, init, (to_chunks(q), to_chunks(k), to_chunks(v), to_chunks(ig), to_chunks(lf)))
    return state, jnp.moveaxis(h, 0, 1).reshape(B, T, H, E)


def dilated_band_prompt(q, k, v, dilation, n_back, slopes):
    B, T, H, E = q.shape
    L = T // dilation
    n_blk = -(-L // BAND_BLOCK)
    Lp = n_blk * BAND_BLOCK
    def to_blocks(a):
        a = a.reshape(B, L, dilation, H, E).transpose(0, 2, 1, 3, 4)
        a = jnp.pad(a, ((0, 0), (0, 0), (0, Lp - L), (0, 0), (0, 0)))
        return a.reshape(B, dilation, n_blk, BAND_BLOCK, H, E)
    def with_prev(a):
        prev = jnp.concatenate([jnp.zeros_like(a[:, :, :1]), a[:, :, :-1]], axis=2)
        return jnp.concatenate([prev, a], axis=3)
    qb = to_blocks(q)
    kb = with_prev(to_blocks(k))
    vb = with_prev(to_blocks(v))
    s = jnp.einsum('bdnqhe,bdnkhe->bdnhqk', qb, kb).astype(jnp.float32) * (E ** -0.5)
    qi = jnp.arange(BAND_BLOCK)[:, None]
    ci = jnp.arange(2 * BAND_BLOCK)[None, :]
    steps = BAND_BLOCK + qi - ci
    blk = jnp.arange(n_blk)[:, None, None]
    valid = (steps >= 0) & (steps <= n_back) & ((blk > 0) | (ci >= BAND_BLOCK))
    bias = -slopes[:, None, None] * (steps * dilation).astype(jnp.float32)
    s = jnp.where(valid[None, None, :, None], s + bias[None, None, None], -jnp.inf)
    m = s.max(axis=-1)
    p = jnp.exp(s - m[..., None])
    l = p.sum(axis=-1)
    o = jnp.einsum('bdnhqk,bdnkhe->bdnqhe', p, vb.astype(jnp.float32))
    def from_blocks(a):
        rest = a.shape[4:]
        a = a.reshape(B, dilation, Lp, *rest)[:, :, :L]
        return jnp.moveaxis(a, 1, 2).reshape(B, T, *rest)
    m = from_blocks(jnp.moveaxis(m, 3, 4))
    l = from_blocks(jnp.moveaxis(l, 3, 4))
    o = from_blocks(o) / l[..., None]
    return o, m, l


def dilated_gather_sample(q, kc, vc, dilation, n_back, slopes):
    B, S, H, E = q.shape
    W = kc.shape[1] - S
    steps = jnp.arange(n_back + 1)
    idx = W + jnp.arange(S)[:, None] - steps[None, :] * dilation
    valid = idx >= 0
    idx = jnp.maximum(idx, 0)
    kg = kc[:, idx]
    vg = vc[:, idx]
    s = jnp.einsum('bshe,bskhe->bhsk', q, kg).astype(jnp.float32) * (E ** -0.5)
    bias = -slopes[:, None, None] * (steps * dilation).astype(jnp.float32)[None, None, :]
    s = jnp.where(valid[None, None], s + bias, -jnp.inf)
    m = s.max(axis=-1)
    p = jnp.exp(s - m[..., None])
    l = p.sum(axis=-1)
    o = jnp.einsum('bhsk,bskhe->bshe', p, vg.astype(jnp.float32)) / jnp.swapaxes(l, 1, 2)[..., None]
    return o, jnp.swapaxes(m, 1, 2), jnp.swapaxes(l, 1, 2)


def combine_by_denominators(results):
    m_all = functools.reduce(jnp.maximum, [r[1] for r in results])
    ws = [r[2] * jnp.exp(r[1] - m_all) for r in results]
    num = functools.reduce(jnp.add, [w[..., None] * r[0] for w, r in zip(ws, results)])
    den = functools.reduce(jnp.add, ws)
    return num / den[..., None]


def multihead_norm(h, center):
    B, T, H, E = h.shape
    h = h.astype(jnp.float32)
    if center:
        h = h - h.mean(axis=-1, keepdims=True)
    h = h * lax.rsqrt(jnp.mean(jnp.square(h), axis=-1, keepdims=True) + EPS)
    return h.reshape(B, T, H * E)


def out_projection(h_m, o_m, h_a, mh_gain, att_gain, w_out):
    hm = multihead_norm(h_m, True) * mh_gain * jax.nn.sigmoid(o_m.astype(jnp.float32))
    ha = multihead_norm(h_a, False) * att_gain
    cat = jnp.concatenate([hm, ha], axis=-1).astype(w_out.dtype)
    return jnp.einsum('btc,cd->btd', cat, w_out)


def hier_moe(x, w_group, b_group, w_router, b_router, w_gate, w_up, w_down):
    g_prob = jax.nn.softmax(jnp.einsum('nd,dg->ng', x, w_group).astype(jnp.float32) + b_group, axis=-1)
    g_w, g_idx = lax.top_k(g_prob, 1)
    e_logits = jnp.einsum('nd,gde->nge', x, w_router).astype(jnp.float32) + b_router
    e_sel = jnp.take_along_axis(e_logits, g_idx[:, :, None], axis=1)[:, 0]
    e_val, e_idx = lax.top_k(e_sel, TOP_K)
    e_w = jax.nn.softmax(e_val, axis=-1) * g_w
    expert_id = g_idx * E_PER_GROUP + e_idx
    gate = jnp.sum(jax.nn.one_hot(expert_id, N_EXPERTS, dtype=jnp.float32) * e_w[..., None], axis=1)
    out = jnp.zeros(x.shape, jnp.float32)
    for e in range(N_EXPERTS):
        h = jax.nn.silu(x @ w_gate[e]) * (x @ w_up[e])
        out = out + gate[:, e:e + 1] * (h @ w_down[e]).astype(jnp.float32)
    return out.astype(x.dtype)


def post_layer(x, y, ln1_g, ln1_b, w_group, b_group, w_router, b_router, w_gate, w_up, w_down, ln2_g, ln2_b):
    x1 = layer_norm(ALPHA * x + y, ln1_g, ln1_b)
    B, T, D = x1.shape
    f = hier_moe(x1.reshape(B * T, D), w_group, b_group, w_router, b_router, w_gate, w_up, w_down).reshape(B, T, D)
    return layer_norm(ALPHA * x1 + f, ln2_g, ln2_b)


def setup_inputs(seed: int = 0) -> dict:
    key = jax.random.key(seed)
    ks = jax.random.split(key, 26)
    f32 = jnp.float32
    W_BUF = min(MAX_WINDOW, PAST_LEN)
    def nrm(k, shape, scale):
        return jax.random.normal(k, shape, f32) * scale
    b_gate = jnp.concatenate([
        nrm(ks[8], (DEPTH, N_MLSTM_HEADS), 0.1),
        jnp.linspace(3.0, 6.0, N_MLSTM_HEADS, dtype=f32)[None, :] + nrm(ks[9], (DEPTH, N_MLSTM_HEADS), 0.1)], axis=-1)
    return {
        'x_prompt': nrm(ks[0], (BATCH, SEQ, D_MODEL), 1.0),
        'x_sample': nrm(ks[1], (DEC_BATCH, DEC_SEQ, D_MODEL), 1.0),
        'state_conv': nrm(ks[2], (DEPTH, DEC_BATCH, CONV_W - 1, 2 * D_MLSTM), 1.0),
        'state_mlstm_C': nrm(ks[3], (DEPTH, DEC_BATCH, N_MLSTM_HEADS, HD_MLSTM, HD_MLSTM), 0.05),
        'state_mlstm_n': nrm(ks[4], (DEPTH, DEC_BATCH, N_MLSTM_HEADS, HD_MLSTM), 0.1),
        'state_mlstm_m': jax.random.uniform(ks[5], (DEPTH, DEC_BATCH, N_MLSTM_HEADS), f32, 0.0, 3.0),
        'cache_win_k': nrm(ks[6], (DEPTH, DEC_BATCH, W_BUF, N_ATT_HEADS, HD_ATT), 1.0),
        'cache_win_v': nrm(ks[7], (DEPTH, DEC_BATCH, W_BUF, N_ATT_HEADS, HD_ATT), 1.0),
        'w_in': nrm(ks[10], (DEPTH, D_MODEL, N_IN_COLS), D_MODEL ** -0.5),
        'b_gate': b_gate,
        'conv_w': nrm(ks[11], (DEPTH, CONV_W, 2 * D_MLSTM), CONV_W ** -0.5),
        'conv_b': nrm(ks[12], (DEPTH, 2 * D_MLSTM), 0.01),
        'mh_gain': 1.0 + nrm(ks[13], (DEPTH, D_MLSTM), 0.02),
        'att_gain': 1.0 + nrm(ks[14], (DEPTH, D_ATT), 0.02),
        'w_out': nrm(ks[15], (DEPTH, D_MIX, D_MODEL), BETA * D_MIX ** -0.5),
        'ln1_g': 1.0 + nrm(ks[16], (DEPTH, D_MODEL), 0.02),
        'ln1_b': nrm(ks[17], (DEPTH, D_MODEL), 0.02),
        'w_group': nrm(ks[18], (DEPTH, D_MODEL, N_GROUPS), D_MODEL ** -0.5),
        'b_group': nrm(ks[19], (DEPTH, N_GROUPS), 0.01),
        'w_router': nrm(ks[20], (DEPTH, N_GROUPS, D_MODEL, E_PER_GROUP), D_MODEL ** -0.5),
        'b_router': nrm(ks[21], (DEPTH, N_GROUPS, E_PER_GROUP), 0.01),
        'w_gate': nrm(ks[22], (DEPTH, N_EXPERTS, D_MODEL, D_FF_EXPERT), D_MODEL ** -0.5),
        'w_up': nrm(ks[23], (DEPTH, N_EXPERTS, D_MODEL, D_FF_EXPERT), D_MODEL ** -0.5),
        'w_down': nrm(ks[24], (DEPTH, N_EXPERTS, D_FF_EXPERT, D_MODEL), BETA * D_FF_EXPERT ** -0.5),
        'ln2_g': 1.0 + nrm(ks[25], (DEPTH, D_MODEL), 0.02),
        'ln2_b': nrm(jax.random.fold_in(ks[25], 1), (DEPTH, D_MODEL), 0.02),
    }


def reference(x_prompt, x_sample, state_conv, state_mlstm_C, state_mlstm_n, state_mlstm_m, cache_win_k, cache_win_v,
              w_in, b_gate, conv_w, conv_b, mh_gain, att_gain, w_out, ln1_g, ln1_b,
              w_group, b_group, w_router, b_router, w_gate, w_up, w_down, ln2_g, ln2_b):
    slopes = alibi_slopes()
    xp, xs = x_prompt, x_sample
    p_conv, p_C, p_n, p_m, p_wk, p_wv = [], [], [], [], [], []
    s_conv, s_C, s_n, s_m, s_wk, s_wv = [], [], [], [], [], []
    for layer in range(DEPTH):
        moe_args = (ln1_g[layer], ln1_b[layer], w_group[layer], b_group[layer], w_router[layer], b_router[layer],
                    w_gate[layer], w_up[layer], w_down[layer], ln2_g[layer], ln2_b[layer])
        T = xp.shape[1]
        qk_raw, v_m, o_m, gates, q_a, k_a, v_a = in_projection(xp, w_in[layer])
        u = jnp.pad(qk_raw, ((0, 0), (CONV_W - 1, 0), (0, 0)))
        q_m, k_m, ig, lf = mlstm_prep(u, gates, conv_w[layer], conv_b[layer], b_gate[layer])
        (C_p, n_p, m_p), h_m = mlstm_prompt(q_m, k_m, v_m.astype(jnp.float32), ig, lf)
        h_a = combine_by_denominators([dilated_band_prompt(q_a, k_a, v_a, d, w // d, slopes) for w, d in DILATED_CONFIGS])
        y = out_projection(h_m, o_m, h_a, mh_gain[layer], att_gain[layer], w_out[layer])
        win = min(MAX_WINDOW, T)
        p_conv.append(u[:, -(CONV_W - 1):])
        p_C.append(C_p)
        p_n.append(n_p)
        p_m.append(m_p)
        p_wk.append(k_a[:, T - win:])
        p_wv.append(v_a[:, T - win:])
        xp = post_layer(xp, y, *moe_args)
        S = xs.shape[1]
        qk_raw, v_m, o_m, gates, q_a, k_a, v_a = in_projection(xs, w_in[layer])
        u = jnp.concatenate([state_conv[layer].astype(qk_raw.dtype), qk_raw], axis=1)
        q_m, k_m, ig, lf = mlstm_prep(u, gates, conv_w[layer], conv_b[layer], b_gate[layer])
        (C_s, n_s, m_s), h_m = mlstm_chunk((state_mlstm_C[layer], state_mlstm_n[layer], state_mlstm_m[layer]),
                                           (q_m, k_m, v_m.astype(jnp.float32), ig, lf))
        kc = jnp.concatenate([cache_win_k[layer].astype(k_a.dtype), k_a], axis=1)
        vc = jnp.concatenate([cache_win_v[layer].astype(v_a.dtype), v_a], axis=1)
        h_a = combine_by_denominators([dilated_gather_sample(q_a, kc, vc, d, w // d, slopes) for w, d in DILATED_CONFIGS])
        y = out_projection(h_m, o_m, h_a, mh_gain[layer], att_gain[layer], w_out[layer])
        s_conv.append(u[:, -(CONV_W - 1):])
        s_C.append(C_s)
        s_n.append(n_s)
        s_m.append(m_s)
        s_wk.append(kc[:, S:])
        s_wv.append(vc[:, S:])
        xs = post_layer(xs, y, *moe_args)
    return (xp, xs,
            jnp.stack(p_conv), jnp.stack(p_C), jnp.stack(p_n), jnp.stack(p_m), jnp.stack(p_wk), jnp.stack(p_wv),
            jnp.stack(s_conv), jnp.stack(s_C), jnp.stack(s_n), jnp.stack(s_m), jnp.stack(s_wk), jnp.stack(s_wv))
```

```python
import math
import numpy as np
import concourse.bass as bass
import concourse.mybir as mybir
from concourse.bass_utils import run_bass_kernel_spmd

F32 = mybir.dt.float32
BF16 = mybir.dt.bfloat16
AF = mybir.ActivationFunctionType
ALU = mybir.AluOpType
AX = mybir.AxisListType

ENGS = ("pe", "act", "dve", "pool", "sp")
N_CORES = 8
E_RUN = 32
STOP = 99


class _Stop(Exception):
    pass
T = 4096
TS = 32
TT = T + TS
NO = 1056
D = 2048
KC = 16
NCOL = 7176
ALPHA = 2.0 ** 0.25
EPS = 1e-5
NEG = -30000.0
ARENA = 53000


class Prog:
    def __init__(self, nc):
        self.nc = nc
        self.ops = []
        self.frozen = False

    def add(self, eng, fn, reads=(), writes=(), dma_sem=None):
        if self.frozen:
            return
        writes = tuple(writes) + tuple(k for k in reads if isinstance(k, tuple) and k[0] == "ps" and k not in writes)
        self.ops.append(dict(eng=eng, fn=fn, reads=tuple(reads), writes=tuple(writes), dma_sem=dma_sem))

    def pe(self, fn, reads=(), writes=()):
        self.add("pe", fn, reads, writes)

    def act(self, fn, reads=(), writes=()):
        self.add("act", fn, reads, writes)

    def dve(self, fn, reads=(), writes=()):
        self.add("dve", fn, reads, writes)

    def dma(self, eng, out, in_, reads=(), writes=(), sem=None, **kw):
        if sem is None:
            sem = ("d",) + tuple(writes[:1] or reads[:1])
        self.add(eng, lambda e: e.dma_start(out=out, in_=in_, **kw), reads, writes, dma_sem=sem)

    def barrier(self):
        if self.frozen:
            return
        self.ops.append(None)

    def build(self):
        nc = self.nc
        segs = [[]]
        for op in self.ops:
            if op is None:
                segs.append([])
            else:
                segs[-1].append(op)
        sems = {}
        sem_ctx = []

        def get_sem(name):
            if name not in sems:
                g = nc.semaphore("s%d" % len(sems))
                sems[name] = g.__enter__()
                sem_ctx.append(g)
            return sems[name]

        eng_cnt = {e: 0 for e in ENGS}
        dma_cnt = {}
        wd = {e: {} for e in ENGS}
        self.n_waits = 0
        for seg in segs:
            ops = seg
            n = len(ops)
            last_writer, readers = {}, {}
            deps = [None] * n
            eng_seq = {e: 0 for e in ENGS}
            seq_of = [0] * n
            waited = {c: {p: -1 for p in ENGS} for c in ENGS}
            signaled = [False] * n
            for i, op in enumerate(ops):
                e = op["eng"]
                seq_of[i] = eng_seq[e]
                eng_seq[e] += 1
                cand = set()
                for k in op["reads"]:
                    w = last_writer.get(k)
                    if w is not None:
                        cand.add((w, True))
                for k in op["writes"]:
                    w = last_writer.get(k)
                    if w is not None:
                        cand.add((w, False))
                    for r in readers.get(k, {}).values():
                        cand.add((r, False))
                dd, best = [], {}
                for (j, raw) in cand:
                    pj = ops[j]
                    if j == i:
                        continue
                    if pj["dma_sem"] is not None:
                        dd.append(j)
                        continue
                    if pj["eng"] == e and not raw:
                        continue
                    if seq_of[j] <= waited[e][pj["eng"]]:
                        continue
                    pe_ = pj["eng"]
                    if pe_ not in best or seq_of[j] > seq_of[best[pe_]]:
                        best[pe_] = j
                for pe_, j in best.items():
                    waited[e][pe_] = seq_of[j]
                    dd.append(j)
                    signaled[j] = True
                deps[i] = sorted(set(dd))
                rk = e if op["dma_sem"] is None else ("dma", i)
                for k in op["reads"]:
                    readers.setdefault(k, {})[rk] = i
                for k in op["writes"]:
                    last_writer[k] = i
                    readers[k] = {}
            sig_val = [None] * n
            for i, op in enumerate(ops):
                if op["dma_sem"] is not None:
                    s = op["dma_sem"]
                    dma_cnt[s] = dma_cnt.get(s, 0) + 16
                    sig_val[i] = (("dma", s), dma_cnt[s])
                elif signaled[i]:
                    eng_cnt[op["eng"]] += 1
                    sig_val[i] = (("eng", op["eng"]), eng_cnt[op["eng"]])
            for i in range(n):
                if sig_val[i] is not None:
                    get_sem(sig_val[i][0])
            per_eng = {e: [i for i in range(n) if ops[i]["eng"] == e] for e in ENGS}
            dma_snapshot = dict(dma_cnt)

            def emit_engine(ename):
                def body(eng):
                    for i in per_eng[ename]:
                        op = ops[i]
                        for j in deps[i]:
                            key, val = sig_val[j]
                            if wd[ename].get(key, 0) >= val:
                                continue
                            wd[ename][key] = val
                            eng.wait_ge(get_sem(key), val)
                            self.n_waits += 1
                        ins = op["fn"](eng)
                        if sig_val[i] is not None:
                            key, val = sig_val[i]
                            ins.then_inc(get_sem(key), 16 if key[0] == "dma" else 1)
                    if ename == "sp":
                        for s, v in dma_snapshot.items():
                            if wd["sp"].get(("dma", s), 0) < v:
                                wd["sp"][("dma", s)] = v
                                eng.wait_ge(get_sem(("dma", s)), v)
                return body

            with nc.Block() as block:
                block.tensor(emit_engine("pe"))
                block.scalar(emit_engine("act"))
                block.vector(emit_engine("dve"))
                block.gpsimd(emit_engine("pool"))
                block.sync(emit_engine("sp"))
        for g in reversed(sem_ctx):
            g.__exit__(None, None, None)


def build_program():
    nc = bass.Bass("TRN2", target_bir_lowering=False)
    P = Prog(nc)

    def din(name, shape):
        return nc.dram_tensor(name, list(shape), F32, kind="ExternalInput").ap()

    def dout(name, shape):
        return nc.dram_tensor(name, list(shape), F32, kind="ExternalOutput").ap()

    xTl_d = din("xTl", [8, 128, KC * 512])
    xTs_d = din("xTs", [128, KC * 32])
    xown_d = din("xown", [NO, D])
    winl_d = din("w_in_l", [56, 128, KC * 128])
    wgt_d = din("wgate8", [D, 8])
    wout_d = din("w_out", [D, D])
    pvec_d = din("pvec", [128, 256])
    srow_d = din("srow", [1, 16])
    lnp_d = din("lnp", [4, 128, D])
    wr_d = din("wr", [D, 36])
    wg_d = din("wg", [32, 8, 128, KC * 128])
    wu_d = din("wu", [32, 8, 128, KC * 128])
    wd_d = din("wd", [32, 1024, D])
    sconv_d = din("sconvT", [4, D, 3])
    sC_d = din("sC", [4, 4, 256, 256])
    sn_d = din("sn", [4, 4, 256])
    ckT_d = din("ckT", [4, 8, 128, 2048])
    ck_d = din("ck", [4, 2048, 8, 128])
    cv_d = din("cv", [4, 2048, 8, 128])
    cvl_d = din("cvl", [4, 8, 128, 16 * 128])
    cst_d = din("cst", [128, 512])
    bt_d = din("bt", [8, 128, 23 * 128])
    bs_d = din("bs", [8, 128, 136])

    y_d = dout("y", [NO, D])
    pconv_d = dout("pconvT", [D, 3])
    pC_d = dout("pC", [4, 256, 256])
    pn_d = dout("pn", [4, 256])
    pm_d = dout("pm", [1, 4])
    pwk_d = dout("pwkT", [8, 128, 2048])
    pwv_d = dout("pwv", [2048, 8, 128])
    oconv_d = dout("oconvT", [4, D, 3])
    oC_d = dout("oC", [4, 4, 256, 256])
    on_d = dout("on", [4, 4, 256])
    om_d = dout("om", [1, 16])
    owk_d = dout("owk", [4, 2048, 8, 128])
    owv_d = dout("owv", [4, 2048, 8, 128])

    with nc.allow_non_contiguous_dma(reason="tiny strided state vectors"), \
            nc.sbuf_tensor("arena", [128, ARENA], F32) as arena:
        pst = []
        pctx = []
        for i in range(8):
            g = nc.psum_tensor("ps%d" % i, [128, 512], F32)
            pst.append(g.__enter__())
            pctx.append(g)
        ps = [t[:] for t in pst]
        psb = [t[:].bitcast(BF16) for t in pst]
        A = arena

        def fv(off, n):
            return A[:, off:off + n]

        def bv(off, n):
            return A[:, off:off + n // 2].bitcast(BF16)

        def v3(ap, b):
            return ap.rearrange("p (a b) -> p a b", b=b)

        class Al:
            def __init__(self, start):
                self.o = start

            def f(self, n):
                o = self.o
                self.o += n
                assert self.o <= ARENA, self.o
                return fv(o, n)

            def b(self, n):
                n2 = (n + 1) // 2 * 2
                o = self.o
                self.o += n2 // 2
                assert self.o <= ARENA, self.o
                return bv(o, n2)[:, 0:n]

        def MM(out, lhsT, rhs, start, stop, rd, wr):
            P.pe(lambda e: e.matmul(out, lhsT=lhsT, rhs=rhs, start=start, stop=stop), rd, wr)

        def TR(out, in_, ident, rd, wr):
            P.pe(lambda e: e.transpose(out, in_, ident), rd, wr)

        def ACT(out, in_, func, rd, wr, **kw):
            P.act(lambda e: e.activation(out=out, in_=in_, func=func, **kw), rd, wr)

        def TS(out, in0, s1, s2, op0, op1, rd, wr):
            if op1 is None:
                P.dve(lambda e: e.tensor_scalar(out=out, in0=in0, scalar1=s1, scalar2=None, op0=op0), rd, wr)
            else:
                P.dve(lambda e: e.tensor_scalar(out=out, in0=in0, scalar1=s1, scalar2=s2, op0=op0, op1=op1), rd, wr)

        def STT(out, in0, scalar, in1, op0, op1, rd, wr):
            P.dve(lambda e: e.scalar_tensor_tensor(out=out, in0=in0, scalar=scalar, in1=in1, op0=op0, op1=op1), rd, wr)

        def TTo(out, in0, in1, op, rd, wr):
            P.dve(lambda e: e.tensor_tensor(out=out, in0=in0, in1=in1, op=op), rd, wr)

        def CP(out, in_, rd, wr):
            P.dve(lambda e: e.tensor_copy(out=out, in_=in_), rd, wr)

        def MS(ap, val, wr):
            P.dve(lambda e: e.memset(ap, val), (), wr)

        def RMAX(out, in_, rd, wr):
            P.dve(lambda e: e.reduce_max(out=out, in_=in_, axis=AX.X), rd, wr)

        def RCP(out, in_, rd, wr):
            P.dve(lambda e: e.reciprocal(out=out, in_=in_), rd, wr)

        pa = Al(0)
        cst = pa.f(512)
        identF = cst[:, 0:128]
        onesF = cst[:, 128:256]
        tri = cst[:, 256:320]
        identB = pa.b(128)
        pvec = pa.f(256)
        cw = v3(pvec[:, 0:64], 4)
        cb = pvec[:, 64:80]
        gain = pvec[:, 80:96]
        sel = pvec[:, 96:100]
        bg = pvec[:, 100:108]
        brt = pvec[:, 108:144]
        smr = pvec[:, 144:160]
        nbg = pa.f(8)
        SG = pa.f(64)
        SG3 = v3(SG, 4)
        esm = pa.f(16)
        wg8 = pa.b(KC * 8)
        wg83 = v3(wg8, 8)
        srow = pa.f(16)
        junk = pa.b(256)
        s1, s2, dm, t0, mean, t1a, nt1, sd, rstd = (pa.f(1) for _ in range(9))
        CAT_OFF = pa.o
        catT = pa.b(KC * NO)
        catT3 = v3(catT, NO)
        P_END = pa.o
        ACC0 = ARENA - (9 * D + KC * NO // 2 + 9 * 32)
        accA = Al(ACC0)
        acc = accA.f(9 * D)
        acc3 = v3(acc, D)
        x1Tb = accA.b(KC * NO)
        x1Tb3 = v3(x1Tb, NO)
        gates = accA.f(9 * 32)
        gates3 = v3(gates, 32)

        def wblk(cb):
            return winl_d[cb].rearrange("p (kc n) -> p kc n", n=128)

        def xblk(blk):
            if blk < 8:
                return xTl_d[blk].rearrange("p (kc t) -> p kc t", t=512)
            return xTs_d.rearrange("p (kc t) -> p kc t", t=32)

        P.dma("sp", cst, cst_d, writes=["cst"])
        P.dma("sp", pvec, pvec_d, writes=["pvec"])
        P.dma("sp", srow[0:1, :], srow_d, writes=["srow"])
        P.dma("pool", wg83, wgt_d.rearrange("(kc p) n -> p kc n", p=128), writes=["wg8"], sem="setup2")
        for b in range(4):
            P.dma("sp", owk_d[b, 0:2040], ck_d[b, 8:2048], writes=[("owk", b)], sem="shift")
            P.dma("sp", owv_d[b, 0:2040], cv_d[b, 8:2048], writes=[("owv", b)], sem="shift")
        CP(identB, identF, ["cst"], ["identB"])
        TS(nbg, bg, -1.0, None, ALU.mult, None, ["pvec"], ["nbg"])
        for s_ in range(4):
            TS(SG3[:, :, s_], gain, sel[:, s_:s_ + 1], None, ALU.mult, None, ["pvec"], ["SG"])
        ACT(esm, smr, AF.Exp, ["pvec"], ["esm"])
        MS(catT, 0.0, ["catT"])
        P.barrier()
        if STOP == 0:
            P.frozen = True

        a1 = Al(P_END)
        Wm = a1.b(KC * 1024)
        Wm3 = v3(Wm, 1024)
        xb = [v3(a1.b(KC * 512), 512) for _ in range(2)]
        U = [a1.f(515) for _ in range(4)]
        Us = [v3(a1.f(44), 11) for _ in range(4)]
        cacc = a1.f(512)
        sgt = a1.f(512)
        QT = [a1.b(512) for _ in range(2)]
        KT = [a1.b(512) for _ in range(2)]
        sigO = v3(a1.b(8 * 256), 256)
        Vf = v3(a1.f(8 * 257), 257)
        G = v3(a1.f(64), 8)
        gz = a1.f(8)
        gsp = a1.f(8)
        gr = a1.f(8)
        ger = a1.f(8)
        genb = a1.f(8)
        geB = a1.f(8)
        gerr = a1.f(8)
        grm = a1.f(1)
        Vp = v3(a1.b(8 * 258), 258)
        Vpp = v3(a1.b(8 * 258), 258)
        Ktok = v3(a1.b(8 * 256), 256)
        PmT = a1.b(64)
        Cf = a1.f(514)
        Cf3 = v3(Cf, 257)
        Cb = a1.b(514)
        Cb3 = v3(Cb, 257)
        Cfs = [a1.f(514) for _ in range(4)]
        Cbs = [a1.b(514) for _ in range(4)]
        hn = a1.f(256)
        hm = a1.b(256)
        Rrow = a1.f(72)
        Brow = a1.f(72)
        mrow = a1.f(8)
        enm = a1.f(8)
        enmb = a1.f(8)
        Cout = a1.f(514)
        Cout3 = v3(Cout, 257)

        def norm_rows(L, psn, W, center, den_key_rd, dm_ap_fn, out_fn, tag):
            num = psn[0:L, 0:W]
            ACT(junk[0:L, 0:W], num, AF.Copy, [tag], ["junk", "s1"], accum_out=s1[0:L, :])
            ACT(junk[0:L, 0:W], num, AF.Square, [tag], ["junk", "s2"], accum_out=s2[0:L, :])
            dm_ap_fn()
            TS(t0[0:L, :], dm[0:L, :], dm[0:L, :], EPS, ALU.mult, ALU.mult, ["dm"], ["t0"])
            if center:
                TS(mean[0:L, :], s1[0:L, :], 1.0 / W, None, ALU.mult, None, ["s1"], ["mean"])
            else:
                MS(mean[0:L, :], 0.0, ["mean"])
            STT(t1a[0:L, :], s2[0:L, :], 1.0 / W, t0[0:L, :], ALU.mult, ALU.add, ["s2", "t0"], ["t1a"])
            STT(nt1[0:L, :], mean[0:L, :], mean[0:L, :], t1a[0:L, :], ALU.mult, ALU.subtract, ["mean", "t1a"], ["nt1"])
            ACT(sd[0:L, :], nt1[0:L, :], AF.Sqrt, ["nt1"], ["sd"], scale=-1.0)
            RCP(rstd[0:L, :], sd[0:L, :], ["sd"], ["rstd"])
            out_fn()

        MS(Vf[:, :, 256:257], 1.0, ["Vf1"])
        for h in range(4):
            for j_, c0 in enumerate((2 * h, 8 + 2 * h, 24 + 2 * h, 16 + 2 * h)):
                for i_ in range(2):
                    P.dma("pool", Wm3[:, :, j_ * 256 + i_ * 128:j_ * 256 + (i_ + 1) * 128], wblk(c0 + i_),
                          writes=["Wm"], sem="Wm")
            for cc in range(4):
                MS(U[cc][:, 0:3], 0.0, [("U", cc)])
            MS(Cf, 0.0, ["Cf"])
            MS(Cb, 0.0, ["Cb"])
            for blk in range(9):
                samp = blk == 8
                N = 32 if samp else 512
                L = 8 if samp else 64
                nch = 4 if samp else 8
                xs = blk % 2
                xk = ("xb", xs)
                P.dma("pool", xb[xs][:, :, 0:N], xblk(blk), writes=[xk], sem=xk)
                for cc in range(4):
                    gcc = 2 * h + cc if cc < 2 else 8 + 2 * h + (cc - 2)
                    pb = cc % 2
                    for kc in range(KC):
                        MM(ps[pb][:, 0:N], Wm3[:, kc, cc * 128:(cc + 1) * 128], xb[xs][:, kc, 0:N],
                           kc == 0, kc == KC - 1, ["Wm", xk], [("ps", pb)])
                    uk = ("U", cc)
                    if not samp:
                        ACT(U[cc][:, 3:3 + N], ps[pb][:, 0:N], AF.Copy, [("ps", pb)], [uk])
                        src = lambda j: U[cc][:, j:j + N]
                        accv = cacc[:, 0:N]
                        sgv = sgt[:, 0:N]
                    else:
                        uk = ("Us", cc)
                        for b in range(4):
                            P.dma("sp", Us[cc][:, b, 0:3], sconv_d[b, gcc * 128:(gcc + 1) * 128, :],
                                  writes=[uk], sem=("Usl", cc))
                        ACT(Us[cc][:, :, 3:11], ps[pb][:, 0:32].rearrange("p (b t) -> p b t", t=8),
                            AF.Copy, [("ps", pb)], [uk])
                        src = lambda j: Us[cc][:, :, j:j + 8]
                        accv = cacc[:, 0:32].rearrange("p (b t) -> p b t", t=8)
                        sgv = sgt[:, 0:32].rearrange("p (b t) -> p b t", t=8)
                    TS(accv, src(3), cw[:, gcc, 3:4], cb[:, gcc:gcc + 1], ALU.mult, ALU.add, [uk, "pvec"], ["cacc"])
                    for j in (2, 1, 0):
                        STT(accv, src(j), cw[:, gcc, j:j + 1], accv, ALU.mult, ALU.add, [uk, "pvec", "cacc"], ["cacc"])
                    ACT(sgv, accv, AF.Sigmoid, ["cacc"], ["sgt"])
                    dst = QT[cc] if cc < 2 else KT[cc - 2]
                    dk = ("QT", cc) if cc < 2 else ("KT", cc - 2)
                    dstv = dst[:, 0:N] if not samp else dst[:, 0:32].rearrange("p (b t) -> p b t", t=8)
                    STT(dstv, accv, 1.0 if cc < 2 else 0.0625, sgv, ALU.mult, ALU.mult, ["cacc", "sgt"], [dk])
                    if not samp:
                        if blk == 7:
                            P.dma("sp", pconv_d[gcc * 128:(gcc + 1) * 128, :], U[cc][:, 512:515],
                                  reads=[uk], writes=[("pconv", gcc)], sem=("pco", cc))
                        CP(U[cc][:, 0:3], U[cc][:, N:N + 3], [uk], [uk])
                    else:
                        for b in range(4):
                            P.dma("sp", oconv_d[b, gcc * 128:(gcc + 1) * 128, :], Us[cc][:, b, 8:11],
                                  reads=[uk], writes=[("oconv", gcc, b)], sem=("oco", cc))
                for j in range(nch):
                    lt = lambda kc: xb[xs][:, kc, j * L:(j + 1) * L]
                    po, pv_ = 2 + j % 2, 4 + j % 2
                    for kc in range(KC):
                        MM(ps[po][0:L, 0:256], lt(kc), Wm3[:, kc, 512:768], kc == 0, kc == KC - 1,
                           ["Wm", xk], [("ps", po)])
                    ACT(sigO[0:L, j, :], ps[po][0:L, 0:256], AF.Sigmoid, [("ps", po)], [("sigO", j)])
                    for kc in range(KC):
                        MM(ps[pv_][0:L, 0:256], lt(kc), Wm3[:, kc, 768:1024], kc == 0, kc == KC - 1,
                           ["Wm", xk], [("ps", pv_)])
                    ACT(Vf[0:L, j, 0:256], ps[pv_][0:L, 0:256], AF.Copy, [("ps", pv_)], [("Vf", j)])
                    for kc in range(KC):
                        MM(ps[6][0:L, j * 8:(j + 1) * 8], lt(kc), wg83[:, kc, :], kc == 0, kc == KC - 1,
                           ["wg8", xk], [("ps", 6)])
                CP(G[0:L, 0:nch, :], ps[6][0:L, 0:nch * 8].rearrange("p (a b) -> p a b", b=8), [("ps", 6)], ["G"])
                igv = G[0:L, 0:nch, h]
                fgv = G[0:L, 0:nch, 4 + h]
                ACT(gz[0:L, 0:nch], fgv, AF.Exp, ["G", "nbg"], ["gz"], scale=-1.0, bias=nbg[0:L, 4 + h:5 + h])
                ACT(gsp[0:L, 0:nch], gz[0:L, 0:nch], AF.Ln, ["gz"], ["gsp"], bias=1.0)
                MM(ps[7][0:L, 0:nch], tri[0:L, 0:L], gsp[0:L, 0:nch], True, True, ["cst", "gsp"], [("ps", 7)])
                MM(ps[7][:, 16:16 + nch], onesF[0:L, :], gsp[0:L, 0:nch], True, True, ["cst", "gsp"], [("ps", 7)])
                STT(gr[0:L, 0:nch], igv, bg[0:L, h:h + 1], ps[7][0:L, 0:nch], ALU.add, ALU.add,
                    ["G", "pvec", ("ps", 7)], ["gr"])
                ACT(ger[0:L, 0:nch], gr[0:L, 0:nch], AF.Exp, ["gr"], ["ger"])
                ACT(genb[0:L, 0:nch], ps[7][0:L, 0:nch], AF.Exp, [("ps", 7)], ["genb"])
                ACT(geB[:, 0:nch], ps[7][:, 16:16 + nch], AF.Exp, [("ps", 7)], ["geB"], scale=-1.0)
                TTo(gerr[0:L, 0:nch], ger[0:L, 0:nch], geB[0:L, 0:nch], ALU.mult, ["ger", "geB"], ["gerr"])
                TS(Brow[0:1, blk * 8:blk * 8 + nch], ps[7][0:1, 16:16 + nch], -1.0, None, ALU.mult, None,
                   [("ps", 7)], ["Brow"])
                TR(ps[7][0:nch, 32:32 + L], gr[0:L, 0:nch], identF[0:L, 0:L], ["gr", "cst"], [("ps", 7)])
                RMAX(grm[0:nch, :], ps[7][0:nch, 32:32 + L], [("ps", 7)], ["grm"])
                TR(ps[7][0:1, 112:112 + nch], grm[0:nch, :], identF[0:nch, 0:nch], ["grm", "cst"], [("ps", 7)])
                CP(Rrow[0:1, blk * 8:blk * 8 + nch], ps[7][0:1, 112:112 + nch], [("ps", 7)], ["Rrow"])
                for j in range(nch):
                    if blk == 0 and h == 0:
                        pass
                    TS(Vp[0:L, j, 0:257], Vf[0:L, j, :], ger[0:L, j:j + 1], None, ALU.mult, None,
                       [("Vf", j), "Vf1", "ger"], [("Vp", j)])
                    ACT(Vpp[0:L, j, 0:257], Vf[0:L, j, :], AF.Copy, [("Vf", j), "Vf1", "gerr"], [("Vpp", j)],
                        scale=gerr[0:L, j:j + 1])
                    pk = 4 + j % 2
                    for ec in range(2):
                        TR(psb[pk][0:L, ec * 128:(ec + 1) * 128], KT[ec][:, j * L:(j + 1) * L], identB,
                           [("KT", ec), "identB"], [("ps", pk)])
                    CP(Ktok[0:L, j, :], psb[pk][0:L, 0:256], [("ps", pk)], [("Ktok", j)])
                if samp:
                    for b in range(4):
                        Cs3 = v3(Cfs[b], 257)
                        P.dma("sp", Cs3[:, :, 0:256], sC_d[b, h].rearrange("(ec p) f -> p ec f", p=128),
                              writes=[("Cfs", b)], sem=("Cfl", b))
                        P.dma("sp", Cs3[:, :, 256:257], sn_d[b, h].rearrange("(ec p one) -> p ec one", p=128, one=1),
                              writes=[("Cfs", b)], sem=("Cfl", b))
                        TS(Cfs[b], Cfs[b], esm[:, b * 4 + h:b * 4 + h + 1], None, ALU.mult, None,
                           [("Cfs", b), "esm"], [("Cfs", b)])
                        ACT(Cbs[b], Cfs[b], AF.Copy, [("Cfs", b)], [("Cbs", b)])
                for j in range(nch):
                    if samp:
                        cf, cbv, cfk, cbk = Cfs[j], Cbs[j], ("Cfs", j), ("Cbs", j)
                    else:
                        cf, cbv, cfk, cbk = Cf, Cb, "Cf", "Cb"
                    cf3, cb3 = v3(cf, 257), v3(cbv, 257)
                    qs = [QT[ec][:, j * L:(j + 1) * L] for ec in range(2)]
                    ks = [KT[ec][:, j * L:(j + 1) * L] for ec in range(2)]
                    for ec in range(2):
                        MM(ps[0][0:L, 0:L], ks[ec], qs[ec], ec == 0, ec == 1,
                           [("KT", ec), ("QT", ec)], [("ps", 0)])
                    TTo(PmT[0:L, 0:L], ps[0][0:L, 0:L], tri[0:L, 0:L], ALU.mult, [("ps", 0), "cst"], ["PmT"])
                    MM(ps[1][0:L, 0:257], PmT[0:L, 0:L], Vp[0:L, j, 0:257], True, False,
                       ["PmT", ("Vp", j)], [("ps", 1)])
                    for ec in range(2):
                        MM(ps[1][0:L, 0:257], qs[ec], cb3[:, ec, :], False, ec == 1,
                           [("QT", ec), cbk], [("ps", 1)])
                    for ec in range(2):
                        MM(ps[2 + ec][:, 0:257], Ktok[0:L, j, ec * 128:(ec + 1) * 128], Vpp[0:L, j, 0:257],
                           True, True, [("Ktok", j), ("Vpp", j)], [("ps", 2 + ec)])
                        STT(cf3[:, ec, :], cf3[:, ec, :], geB[:, j:j + 1], ps[2 + ec][:, 0:257], ALU.mult, ALU.add,
                            [cfk, "geB", ("ps", 2 + ec)], [cfk])
                    ACT(cbv, cf, AF.Copy, [cfk], [cbk])

                    def dmf(L=L, j=j):
                        ACT(t0[0:L, :], ps[1][0:L, 256:257], AF.Abs, [("ps", 1)], ["t0"])
                        TS(dm[0:L, :], t0[0:L, :], genb[0:L, j:j + 1], None, ALU.max, None, ["t0", "genb"], ["dm"])

                    def outf(L=L, j=j):
                        TS(hn[0:L, :], ps[1][0:L, 0:256], mean[0:L, :], rstd[0:L, :], ALU.subtract, ALU.mult,
                           [("ps", 1), "mean", "rstd"], ["hn"])
                        TTo(hm[0:L, :], hn[0:L, :], sigO[0:L, j, :], ALU.mult, ["hn", ("sigO", j)], ["hm"])

                    norm_rows(L, ps[1], 256, True, None, dmf, outf, ("ps", 1))
                    for fh in range(2):
                        TR(psb[6][:, fh * 64:fh * 64 + L], hm[0:L, fh * 128:(fh + 1) * 128], identB[0:L, 0:L],
                           ["hm", "identB"], [("ps", 6)])
                        fc = 2 * h + fh
                        if not samp:
                            col = (blk % 2) * 512 + j * 64
                            STT(catT3[:, fc, col:col + 64], psb[6][:, fh * 64:fh * 64 + 64], SG3[:, fc, blk // 2:blk // 2 + 1],
                                catT3[:, fc, col:col + 64], ALU.mult, ALU.add, [("ps", 6), "SG", "catT"], ["catT"])
                        else:
                            col = 1024 + 8 * j
                            TS(catT3[:, fc, col:col + 8], psb[6][:, fh * 64:fh * 64 + 8], gain[:, fc:fc + 1], None,
                               ALU.mult, None, [("ps", 6), "pvec"], ["catT"])
            MS(mrow[0:1, 0:1], 0.0, ["mrow"])
            for c in range(64):
                STT(mrow[0:1, 0:1], mrow[0:1, 0:1], Rrow[0:1, c:c + 1], Brow[0:1, c:c + 1], ALU.max, ALU.add,
                    ["mrow", "Rrow", "Brow"], ["mrow"])
            TTo(mrow[0:1, 4:8], srow[0:1, :].rearrange("p (b h) -> p b h", h=4)[:, :, h], Rrow[0:1, 64:68], ALU.max,
                ["srow", "Rrow"], ["mrow2"])
            TTo(mrow[0:1, 4:8], mrow[0:1, 4:8], Brow[0:1, 64:68], ALU.add, ["mrow2", "Brow"], ["mrow2"])
            P.dma("sp", pm_d[0:1, h:h + 1], mrow[0:1, 0:1], reads=["mrow"], writes=[("pm", h)], sem="pmo1")
            P.dma("sp", om_d[0:1, :].rearrange("p (b h) -> p b h", h=4)[:, :, h], mrow[0:1, 4:8],
                  reads=["mrow2"], writes=[("om", h)], sem="pmo2")
            ACT(enm[0:1, 0:1], mrow[0:1, 0:1], AF.Exp, ["mrow"], ["enm"], scale=-1.0)
            ACT(enm[0:1, 4:8], mrow[0:1, 4:8], AF.Exp, ["mrow2"], ["enm"], scale=-1.0)
            MM(ps[7][:, 0:8], onesF[0:1, :], enm[0:1, 0:8], True, True, ["cst", "enm"], [("ps", 7)])
            CP(enmb[:, 0:8], ps[7][:, 0:8], [("ps", 7)], ["enmb"])
            TS(Cout, Cf, enmb[:, 0:1], None, ALU.mult, None, ["Cf", "enmb"], ["Cout"])
            P.dma("sp", pC_d[h].rearrange("(ec p) f -> p ec f", p=128), Cout3[:, :, 0:256],
                  reads=["Cout"], writes=[("pC", h)], sem="Co")
            P.dma("sp", pn_d[h].rearrange("(ec p one) -> p ec one", p=128, one=1), Cout3[:, :, 256:257],
                  reads=["Cout"], writes=[("pn", h)], sem="Co")
            for b in range(4):
                TS(Cout, Cfs[b], enmb[:, 4 + b:5 + b], None, ALU.mult, None, [("Cfs", b), "enmb"], ["Cout"])
                P.dma("sp", oC_d[b, h].rearrange("(ec p) f -> p ec f", p=128), Cout3[:, :, 0:256],
                      reads=["Cout"], writes=[("oC", h, b)], sem="Co")
                P.dma("sp", on_d[b, h].rearrange("(ec p one) -> p ec one", p=128, one=1), Cout3[:, :, 256:257],
                      reads=["Cout"], writes=[("on", h, b)], sem="Co")
        P.barrier()
        if STOP == 1:
            P.frozen = True

        a2 = Al(P_END)
        Wa3 = v3(a2.b(KC * 384), 384)
        xb2 = [v3(a2.b(KC * 512), 512) for _ in range(2)]
        KTa = a2.b(TT)
        QTa = a2.b(TT)
        Va = v3(a2.b(32 * 130), 130)
        Vs = v3(a2.b(4 * 130), 130)
        BT = a2.f(23 * 128)
        BS = a2.f(136)
        stg = [a2.f(512) for _ in range(2)]
        stv = [a2.f(128) for _ in range(2)]
        stk = a2.f(128)
        tS = [a2.f(512) for _ in range(2)]
        PT = [a2.b(512) for _ in range(2)]
        kcT = [a2.b(2056) for _ in range(2)]
        vc = [v3(a2.b(16 * 130), 130) for _ in range(2)]
        ha = a2.b(128)
        inv_sqrt_e = 1.0 / math.sqrt(128.0)
        MS(Va[:, :, 128:129], 1.0, ["Va1"])
        MS(Vs[:, :, 128:129], 1.0, ["Vs1"])
        for s_ in range(2):
            MS(vc[s_][:, :, 128:129], 1.0, [("vc1", s_)])

        def att_norm(L, psn, out_cols, scal, is_sel, fc, tag):
            def dmf():
                CP(dm[0:L, :], psn[0:L, 128:129], [tag], ["dm"])

            def outf():
                ACT(ha[0:L, :], psn[0:L, 0:128], AF.Copy, [tag, "rstd"], ["ha"], scale=rstd[0:L, :])

            norm_rows(L, psn, 128, False, None, dmf, outf, tag)
            TR(psb[7][:, 0:L], ha[0:L, :], identB[0:L, 0:L], ["ha", "identB"], [("ps", 7)])
            if is_sel:
                STT(out_cols, psb[7][:, 0:L], scal, out_cols, ALU.mult, ALU.add, [("ps", 7), "SG", "catT"], ["catT"])
            else:
                TS(out_cols, psb[7][:, 0:L], scal, None, ALU.mult, None, [("ps", 7), "pvec"], ["catT"])

        ci = 0
        for a in range(8):
            for j_, c0 in enumerate((32 + a, 40 + a, 48 + a)):
                P.dma("pool", Wa3[:, :, j_ * 128:(j_ + 1) * 128], wblk(c0), writes=["Wa"], sem="Wa")
            P.dma("sp", BT, bt_d[a], writes=["BT"])
            P.dma("sp", BS, bs_d[a], writes=["BS"])
            fc = 8 + a
            for blk in range(9):
                if STOP == 19 + blk and a == 0:
                    P.frozen = True
                samp = blk == 8
                N = 32 if samp else 512
                xs = blk % 2
                xk = ("xb", xs)
                P.dma("pool", xb2[xs][:, :, 0:N], xblk(blk), writes=[xk], sem=xk)
                for kc in range(KC):
                    MM(ps[0][:, 0:N], Wa3[:, kc, 128:256], xb2[xs][:, kc, 0:N], kc == 0, kc == KC - 1,
                       ["Wa", xk], [("ps", 0)])
                ACT(KTa[:, blk * 512:blk * 512 + N], ps[0][:, 0:N], AF.Copy, [("ps", 0)], ["KTa"])
                if 4 <= blk < 8:
                    sg_ = blk % 2
                    CP(stg[sg_], ps[0][:, 0:512], [("ps", 0)], [("stg", sg_)])
                    P.dma("sp", pwk_d[a][:, (blk - 4) * 512:(blk - 3) * 512], stg[sg_], reads=[("stg", sg_)],
                          writes=[("pwk", a, blk)], sem=("stg", sg_))
                for kc in range(KC):
                    MM(ps[1][:, 0:N], Wa3[:, kc, 0:128], xb2[xs][:, kc, 0:N], kc == 0, kc == KC - 1,
                       ["Wa", xk], [("ps", 1)])
                ACT(QTa[:, blk * 512:blk * 512 + N], ps[1][:, 0:N], AF.Copy, [("ps", 1)], ["QTa"], scale=inv_sqrt_e)
                if not samp:
                    for tt in range(4):
                        pv_ = 2 + tt % 2
                        for kc in range(KC):
                            MM(ps[pv_][:, 0:128], xb2[xs][:, kc, tt * 128:(tt + 1) * 128], Wa3[:, kc, 256:384],
                               kc == 0, kc == KC - 1, ["Wa", xk], [("ps", pv_)])
                        ACT(Va[:, blk * 4 + tt, 0:128], ps[pv_][:, 0:128], AF.Copy, [("ps", pv_)], ["Va"])
                        if blk >= 4:
                            sv = tt % 2
                            CP(stv[sv], ps[pv_][:, 0:128], [("ps", pv_)], [("stv", sv)])
                            r0 = (blk - 4) * 512 + tt * 128
                            P.dma("sp", pwv_d[r0:r0 + 128, a, :], stv[sv], reads=[("stv", sv)],
                                  writes=[("pwv", a, blk, tt)], sem=("stv", sv))
                else:
                    for b in range(4):
                        for (wsl, which) in ((slice(256, 384), "v"), (slice(128, 256), "k")):
                            pv_ = 2 + b % 2
                            for kc in range(KC):
                                MM(ps[pv_][0:8, 0:128], xb2[xs][:, kc, b * 8:(b + 1) * 8], Wa3[:, kc, wsl],
                                   kc == 0, kc == KC - 1, ["Wa", xk], [("ps", pv_)])
                            if which == "v":
                                ACT(Vs[0:8, b, 0:128], ps[pv_][0:8, 0:128], AF.Copy, [("ps", pv_)], [("Vs", b)])
                            CP(stk[0:8, :], ps[pv_][0:8, 0:128], [("ps", pv_)], ["stk"])
                            dst = (owv_d if which == "v" else owk_d)[b, 2040:2048, a, :]
                            P.dma("sp", dst, stk[0:8, :], reads=["stk"], writes=[("onew", which, a, b)], sem="stk")
            if STOP == 10:
                P.frozen = True
            for g in range(8):
                kts = list(range(max(0, 4 * g - 16), 4 * g + 4))
                for kt in kts:
                    o0 = 4 * g - kt
                    sb_ = ci % 2
                    ci += 1
                    MM(ps[sb_][:, 0:512], KTa[:, kt * 128:(kt + 1) * 128], QTa[:, g * 512:(g + 1) * 512], True, True,
                       ["KTa", "QTa"], [("ps", sb_)])
                    TTo(tS[sb_], ps[sb_][:, 0:512], BT[:, (o0 + 3) * 128:(o0 + 7) * 128], ALU.add,
                        [("ps", sb_), "BT"], [("tS", sb_)])
                    ACT(PT[sb_], tS[sb_], AF.Exp, [("tS", sb_)], [("PT", sb_)])
                    for qb in range(4):
                        o = o0 + qb
                        if 0 <= o <= 16:
                            qi = 4 * g + qb
                            MM(ps[2 + qb][:, 0:129], PT[sb_][:, qb * 128:(qb + 1) * 128], Va[:, kt, 0:129],
                               kt == max(0, qi - 16), kt == qi, [("PT", sb_), "Va", "Va1"], [("ps", 2 + qb)])
                for qb in range(4):
                    qi = 4 * g + qb
                    col = (qi % 8) * 128
                    att_norm(128, ps[2 + qb], catT3[:, fc, col:col + 128], SG3[:, fc, qi // 8:qi // 8 + 1], True, fc,
                             ("ps", 2 + qb))
            if STOP == 11:
                P.frozen = True
            for b in range(4):
                s_ = b % 2
                P.dma("pool", kcT[s_][:, 0:2048], ckT_d[b, a], writes=[("kcT", s_)], sem=("kcT", s_))
                P.dma("pool", vc[s_][:, :, 0:128], cvl_d[b, a].rearrange("p (t e) -> p t e", e=128),
                      writes=[("vc", s_)], sem=("vc", s_))
                qcols = QTa[:, T + 8 * b:T + 8 * b + 8]
                CP(kcT[s_][:, 2048:2056], KTa[:, T + 8 * b:T + 8 * b + 8], ["KTa"], [("kcT", s_)])
                for tl in range(16):
                    MM(ps[0][:, tl * 8:(tl + 1) * 8], kcT[s_][:, tl * 128:(tl + 1) * 128], qcols, True, True,
                       [("kcT", s_), "QTa"], [("ps", 0)])
                MM(ps[0][0:8, 128:136], kcT[s_][:, 2048:2056], qcols, True, True, [("kcT", s_), "QTa"], [("ps", 0)])
                TTo(tS[0][:, 0:128], ps[0][:, 0:128], BS[:, 0:128], ALU.add, [("ps", 0), "BS"], [("tS", 0)])
                TTo(tS[0][0:8, 128:136], ps[0][0:8, 128:136], BS[0:8, 128:136], ALU.add, [("ps", 0), "BS"], [("tS", 0)])
                ACT(PT[0][:, 0:128], tS[0][:, 0:128], AF.Exp, [("tS", 0)], [("PT", 0)])
                ACT(PT[0][0:8, 128:136], tS[0][0:8, 128:136], AF.Exp, [("tS", 0)], [("PT", 0)])
                for tl in range(16):
                    MM(ps[1][0:8, 0:129], PT[0][:, tl * 8:(tl + 1) * 8], vc[s_][:, tl, 0:129], tl == 0, False,
                       [("PT", 0), ("vc", s_), ("vc1", s_)], [("ps", 1)])
                MM(ps[1][0:8, 0:129], PT[0][0:8, 128:136], Vs[0:8, b, 0:129], False, True,
                   [("PT", 0), ("Vs", b), "Vs1"], [("ps", 1)])
                col = 1024 + 8 * b
                att_norm(8, ps[1], catT3[:, fc, col:col + 8], gain[:, fc:fc + 1], False, fc, ("ps", 1))
        P.barrier()
        if STOP == 2:
            P.frozen = True

        a3 = Al(P_END)
        Wo3 = v3(a3.b(KC * 512), 512)
        ln1g = a3.f(D)
        ln1b = a3.f(D)
        xo = [a3.f(512) for _ in range(2)]
        x1t = a3.f(D)
        x1Tf = a3.f(KC * 128)
        x1Tf3 = v3(x1Tf, 128)
        Wr3 = v3(a3.f(KC * 36), 36)
        lg = a3.f(36)
        gmax = a3.f(1)
        ngmax = a3.f(1)
        gm = a3.f(4)
        gex = a3.f(4)
        gsum = a3.f(1)
        gw = a3.f(1)
        pen = a3.f(4)
        em = a3.f(32)
        em2 = a3.f(32)
        mk1 = a3.f(32)
        mk2 = a3.f(32)
        m1 = a3.f(1)
        m2 = a3.f(1)
        dd_ = a3.f(1)
        w1 = a3.f(1)
        w1g = a3.f(1)
        w2g = a3.f(1)
        gt = a3.f(32)
        l_s1 = a3.f(1)
        l_s2 = a3.f(1)
        l_mean = a3.f(1)
        l_t1 = a3.f(1)
        l_nt = a3.f(1)
        l_sd = a3.f(1)
        l_rstd = a3.f(1)
        jk2 = a3.b(D)
        P.dma("sp", ln1g, lnp_d[0], writes=["ln1g"])
        P.dma("sp", ln1b, lnp_d[1], writes=["ln1b"])
        P.dma("sp", Wr3, wr_d.rearrange("(kc p) n -> p kc n", p=128), writes=["Wr"])
        woutv = wout_d.rearrange("(kc p) n -> p kc n", p=128)
        xi = 0
        for cg in range(4):
            P.dma("pool", Wo3, woutv[:, :, cg * 512:(cg + 1) * 512], writes=["Wo"], sem="Wo")
            for tt in range(9):
                Lr = 128 if tt < 8 else 32
                xs = xi % 2
                xi += 1
                P.dma("sp", xo[xs][0:Lr, :], xown_d[tt * 128:tt * 128 + Lr, cg * 512:(cg + 1) * 512],
                      writes=[("xo", xs)], sem=("xo", xs))
                pb = tt % 2
                for kc in range(KC):
                    MM(ps[pb][0:Lr, :], catT3[:, kc, tt * 128:tt * 128 + Lr], Wo3[:, kc, :], kc == 0, kc == KC - 1,
                       ["catT", "Wo"], [("ps", pb)])
                STT(acc3[0:Lr, tt, cg * 512:(cg + 1) * 512], xo[xs][0:Lr, :], ALPHA, ps[pb][0:Lr, :], ALU.mult, ALU.add,
                    [("xo", xs), ("ps", pb)], [("acc", tt)])

        def layer_norm(Lr, src, srck, gtile, btile, gk, bk, dst, dstk):
            ACT(jk2[0:Lr, :], src, AF.Copy, [srck], ["jk2", "l_s1"], accum_out=l_s1[0:Lr, :])
            ACT(jk2[0:Lr, :], src, AF.Square, [srck], ["jk2", "l_s2"], accum_out=l_s2[0:Lr, :])
            TS(l_mean[0:Lr, :], l_s1[0:Lr, :], 1.0 / D, None, ALU.mult, None, ["l_s1"], ["l_mean"])
            TS(l_t1[0:Lr, :], l_s2[0:Lr, :], 1.0 / D, EPS, ALU.mult, ALU.add, ["l_s2"], ["l_t1"])
            STT(l_nt[0:Lr, :], l_mean[0:Lr, :], l_mean[0:Lr, :], l_t1[0:Lr, :], ALU.mult, ALU.subtract,
                ["l_mean", "l_t1"], ["l_nt"])
            ACT(l_sd[0:Lr, :], l_nt[0:Lr, :], AF.Sqrt, ["l_nt"], ["l_sd"], scale=-1.0)
            RCP(l_rstd[0:Lr, :], l_sd[0:Lr, :], ["l_sd"], ["l_rstd"])
            TS(dst, src, l_mean[0:Lr, :], l_rstd[0:Lr, :], ALU.subtract, ALU.mult, [srck, "l_mean", "l_rstd"], [dstk])
            TTo(dst, dst, gtile[0:Lr, :], ALU.mult, [dstk, gk], [dstk])
            TTo(dst, dst, btile[0:Lr, :], ALU.add, [dstk, bk], [dstk])

        for tt in range(9):
            Lr = 128 if tt < 8 else 32
            c0 = tt * 128
            layer_norm(Lr, acc3[0:Lr, tt, :], ("acc", tt), ln1g, ln1b, "ln1g", "ln1b", x1t[0:Lr, :], "x1t")
            ACT(acc3[0:Lr, tt, :], x1t[0:Lr, :], AF.Copy, ["x1t"], [("acc", tt)], scale=ALPHA)
            for k4 in range(4):
                pb = 2 + k4 % 2
                for q in range(4):
                    kc = k4 * 4 + q
                    TR(ps[pb][:, q * 128:q * 128 + Lr], x1t[0:Lr, kc * 128:(kc + 1) * 128], identF[0:Lr, 0:Lr],
                       ["x1t", "cst"], [("ps", pb)])
                src4 = ps[pb][:, :].rearrange("p (a b) -> p a b", b=128)[:, :, 0:Lr]
                CP(x1Tf3[:, k4 * 4:(k4 + 1) * 4, 0:Lr], src4, [("ps", pb)], ["x1Tf"])
                ACT(x1Tb3[:, k4 * 4:(k4 + 1) * 4, c0:c0 + Lr], src4, AF.Copy, [("ps", pb)], ["x1Tb"])
            for kc in range(KC):
                MM(ps[4][0:Lr, 0:36], x1Tf3[:, kc, 0:Lr], Wr3[:, kc, :], kc == 0, kc == KC - 1, ["x1Tf", "Wr"], [("ps", 4)])
            R = slice(0, Lr)
            TTo(lg[R, :], ps[4][R, 0:36], brt[R, :], ALU.add, [("ps", 4), "pvec"], ["lg"])
            RMAX(gmax[R, :], lg[R, 0:4], ["lg"], ["gmax"])
            TS(gm[R, :], lg[R, 0:4], gmax[R, :], None, ALU.is_equal, None, ["lg", "gmax"], ["gm"])
            TS(ngmax[R, :], gmax[R, :], -1.0, None, ALU.mult, None, ["gmax"], ["ngmax"])
            ACT(gex[R, :], lg[R, 0:4], AF.Exp, ["lg", "ngmax"], ["gex", "gsum"], bias=ngmax[R, :], accum_out=gsum[R, :])
            RCP(gw[R, :], gsum[R, :], ["gsum"], ["gw"])
            TS(pen[R, :], gm[R, :], -1.0, 1e30, ALU.add, ALU.mult, ["gm"], ["pen"])
            for g_ in range(4):
                TS(em[R, g_ * 8:(g_ + 1) * 8], lg[R, 4 + g_ * 8:12 + g_ * 8], pen[R, g_:g_ + 1], None, ALU.add, None,
                   ["lg", "pen"], ["em"])
            RMAX(m1[R, :], em[R, :], ["em"], ["m1"])
            TS(mk1[R, :], em[R, :], m1[R, :], None, ALU.is_equal, None, ["em", "m1"], ["mk1"])
            STT(em2[R, :], mk1[R, :], -1e30, em[R, :], ALU.mult, ALU.add, ["mk1", "em"], ["em2"])
            RMAX(m2[R, :], em2[R, :], ["em2"], ["m2"])
            TS(mk2[R, :], em2[R, :], m2[R, :], None, ALU.is_equal, None, ["em2", "m2"], ["mk2"])
            TTo(dd_[R, :], m1[R, :], m2[R, :], ALU.subtract, ["m1", "m2"], ["dd"])
            ACT(w1[R, :], dd_[R, :], AF.Sigmoid, ["dd"], ["w1"])
            TTo(w1g[R, :], w1[R, :], gw[R, :], ALU.mult, ["w1", "gw"], ["w1g"])
            TTo(w2g[R, :], gw[R, :], w1g[R, :], ALU.subtract, ["gw", "w1g"], ["w2g"])
            TS(gt[R, :], mk1[R, :], w1g[R, :], None, ALU.mult, None, ["mk1", "w1g"], ["gt"])
            STT(gates3[R, tt, :], mk2[R, :], w2g[R, :], gt[R, :], ALU.mult, ALU.add, ["mk2", "w2g", "gt"], ["gates"])
        assert a3.o <= ACC0, (a3.o, ACC0)
        P.barrier()
        if STOP == 3:
            P.frozen = True

        a4 = Al(CAT_OFF)
        WGs = [v3(a4.b(KC * 128), 128) for _ in range(3)]
        WUs = [v3(a4.b(KC * 128), 128) for _ in range(3)]
        WDs = [v3(a4.b(8 * 512), 512) for _ in range(3)]
        hT3 = v3(a4.b(8 * NO), NO)
        sg2 = [a4.b(352) for _ in range(2)]
        ln2g = a4.f(D)
        ln2b = a4.f(D)
        ost = a4.f(D)
        jk3 = a4.b(D)
        m_s1 = a4.f(1)
        P.dma("sp", ln2g, lnp_d[2], writes=["ln2g"])
        P.dma("sp", ln2b, lnp_d[3], writes=["ln2b"])
        gi = 0
        di = 0
        oi = 0
        for e in range(E_RUN):
            for ffc in range(8):
                sl = gi % 3
                gi += 1
                P.dma("pool", WGs[sl], wg_d[e, ffc].rearrange("p (kc n) -> p kc n", n=128), writes=[("WG", sl)], sem=("WG", sl))
                P.dma("pool", WUs[sl], wu_d[e, ffc].rearrange("p (kc n) -> p kc n", n=128), writes=[("WU", sl)], sem=("WU", sl))
                for tg in range(3):
                    pg, pu = (0, 1) if (ffc * 3 + tg) % 2 == 0 else (2, 3)
                    cols = slice(tg * 352, (tg + 1) * 352)
                    for kc in range(KC):
                        MM(ps[pg][:, 0:352], WGs[sl][:, kc, :], x1Tb3[:, kc, cols], kc == 0, kc == KC - 1,
                           [("WG", sl), "x1Tb"], [("ps", pg)])
                    for kc in range(KC):
                        MM(ps[pu][:, 0:352], WUs[sl][:, kc, :], x1Tb3[:, kc, cols], kc == 0, kc == KC - 1,
                           [("WU", sl), "x1Tb"], [("ps", pu)])
                    s2_ = (ffc * 3 + tg) % 2
                    ACT(sg2[s2_], ps[pg][:, 0:352], AF.Silu, [("ps", pg)], [("sg2", s2_)])
                    TTo(hT3[:, ffc, cols], sg2[s2_], ps[pu][:, 0:352], ALU.mult, [("sg2", s2_), ("ps", pu)], [("hT", ffc)])
            wdv = wd_d[e].rearrange("(fc p) n -> p fc n", p=128)
            for cg in range(4):
                sl = di % 3
                di += 1
                P.dma("pool", WDs[sl], wdv[:, :, cg * 512:(cg + 1) * 512], writes=[("WD", sl)], sem=("WD", sl))
                for tt in range(9):
                    Lr = 128 if tt < 8 else 32
                    po = 4 + oi % 4
                    oi += 1
                    for ffc in range(8):
                        MM(ps[po][0:Lr, :], hT3[:, ffc, tt * 128:tt * 128 + Lr], WDs[sl][:, ffc, :], ffc == 0, ffc == 7,
                           [("hT", ffc), ("WD", sl)], [("ps", po)])
                    dst = acc3[0:Lr, tt, cg * 512:(cg + 1) * 512]
                    STT(dst, ps[po][0:Lr, :], gates3[0:Lr, tt, e:e + 1], dst, ALU.mult, ALU.add,
                        [("ps", po), "gates", ("acc", tt, cg)], [("acc", tt, cg)])
        jk2 = jk3
        l_s1, l_s2, l_mean, l_t1, l_nt, l_sd, l_rstd = (a4.f(1) for _ in range(7))
        assert a4.o <= ACC0, (a4.o, ACC0)
        for tt in range(9):
            Lr = 128 if tt < 8 else 32
            srck = [("acc", tt, cg) for cg in range(4)]
            src = acc3[0:Lr, tt, :]
            ACT(jk2[0:Lr, :], src, AF.Copy, srck, ["jk2", "l_s1"], accum_out=l_s1[0:Lr, :])
            ACT(jk2[0:Lr, :], src, AF.Square, srck, ["jk2", "l_s2"], accum_out=l_s2[0:Lr, :])
            TS(l_mean[0:Lr, :], l_s1[0:Lr, :], 1.0 / D, None, ALU.mult, None, ["l_s1"], ["l_mean"])
            TS(l_t1[0:Lr, :], l_s2[0:Lr, :], 1.0 / D, EPS, ALU.mult, ALU.add, ["l_s2"], ["l_t1"])
            STT(l_nt[0:Lr, :], l_mean[0:Lr, :], l_mean[0:Lr, :], l_t1[0:Lr, :], ALU.mult, ALU.subtract,
                ["l_mean", "l_t1"], ["l_nt"])
            ACT(l_sd[0:Lr, :], l_nt[0:Lr, :], AF.Sqrt, ["l_nt"], ["l_sd"], scale=-1.0)
            RCP(l_rstd[0:Lr, :], l_sd[0:Lr, :], ["l_sd"], ["l_rstd"])
            TS(ost[0:Lr, :], src, l_mean[0:Lr, :], l_rstd[0:Lr, :], ALU.subtract, ALU.mult, srck + ["l_mean", "l_rstd"], ["ost"])
            TTo(ost[0:Lr, :], ost[0:Lr, :], ln2g[0:Lr, :], ALU.mult, ["ost", "ln2g"], ["ost"])
            TTo(ost[0:Lr, :], ost[0:Lr, :], ln2b[0:Lr, :], ALU.add, ["ost", "ln2b"], ["ost"])
            P.dma("sp", y_d[tt * 128:tt * 128 + Lr, :], ost[0:Lr, :], reads=["ost"], writes=[("y", tt)], sem="yo")
        P.build()
        for g in reversed(pctx):
            g.__exit__(None, None, None)
    return nc, P


_CACHE = {}


def _tables():
    slopes = np.array([2.0 ** (-8.0 * (h + 1) / 8) for h in range(8)], np.float64)

    def lbias(delta, a):
        cnt = ((delta >= 0) & (delta <= 128)).astype(np.float64)
        cnt += ((delta >= 0) & (delta % 4 == 0) & (delta <= 512))
        cnt += ((delta >= 0) & (delta % 16 == 0) & (delta <= 2048))
        out = np.full(delta.shape, NEG, np.float64)
        ok = cnt > 0
        out[ok] = -slopes[a] * delta[ok] + np.log(cnt[ok])
        return out

    bt = np.zeros((8, 128, 23, 128), np.float32)
    kr = np.arange(128)[:, None]
    qc = np.arange(128)[None, :]
    for a in range(8):
        for oi in range(23):
            o = oi - 3
            bt[a, :, oi, :] = lbias(128 * o + qc - kr, a)
    bs = np.full((8, 128, 136), NEG, np.float32)
    q8 = np.arange(8)[None, :]
    for a in range(8):
        for tl in range(16):
            bs[a, :, tl * 8:(tl + 1) * 8] = lbias(2048 + q8 - (tl * 128 + kr), a)
        bs[a, 0:8, 128:136] = lbias(q8 - np.arange(8)[:, None], a)
    cst = np.zeros((128, 512), np.float32)
    cst[:, 0:128] = np.eye(128, dtype=np.float32)
    cst[:, 128:256] = 1.0
    cst[0:64, 256:320] = np.triu(np.ones((64, 64), np.float32))
    return bt.reshape(8, 128, 23 * 128), bs, cst


def kernel(**inp):
    f = lambda k: np.asarray(inp[k], np.float32)
    xp, xsm = f("x_prompt"), f("x_sample")
    if "nc" not in _CACHE:
        _CACHE["nc"] = build_program()
    nc, P = _CACHE["nc"]
    bt, bs, cst = _tables()
    w_in = f("w_in")[0]
    w_in_l = np.ascontiguousarray(np.concatenate([w_in[:, 0:4096], w_in[:, 4104:7176]], axis=1)
                                  .reshape(KC, 128, 56, 128).transpose(2, 1, 0, 3)).reshape(56, 128, KC * 128)
    wgate8 = np.ascontiguousarray(w_in[:, 4096:4104])
    w_out = np.ascontiguousarray(f("w_out")[0])
    wg = np.ascontiguousarray(f("w_gate")[0].reshape(32, KC, 128, 8, 128).transpose(0, 3, 2, 1, 4)).reshape(32, 8, 128, KC * 128)
    wu = np.ascontiguousarray(f("w_up")[0].reshape(32, KC, 128, 8, 128).transpose(0, 3, 2, 1, 4)).reshape(32, 8, 128, KC * 128)
    wd = np.ascontiguousarray(f("w_down")[0])
    wr = np.ascontiguousarray(np.concatenate([f("w_group")[0], f("w_router")[0].transpose(1, 0, 2).reshape(D, 32)], axis=1))
    lnp = np.stack([np.broadcast_to(f(k)[0][None, :], (128, D)) for k in ("ln1_g", "ln1_b", "ln2_g", "ln2_b")]).astype(np.float32)
    conv_w, conv_b = f("conv_w")[0], f("conv_b")[0]
    gain = np.concatenate([f("mh_gain")[0], f("att_gain")[0]])
    brt = np.concatenate([f("b_group")[0], f("b_router")[0].reshape(32)])
    in_maps = []
    ncores = _CACHE.get("ncores", N_CORES)
    for c in range(ncores):
        b, s = c // 4, c % 4
        sb = slice(4 * c, 4 * c + 4)
        xs_flat = xsm[sb].reshape(32, D)
        pvec = np.zeros((128, 256), np.float32)
        pvec[:, 0:64] = conv_w.reshape(4, KC, 128).transpose(2, 1, 0).reshape(128, 64)
        pvec[:, 64:80] = conv_b.reshape(KC, 128).T
        pvec[:, 80:96] = gain.reshape(KC, 128).T
        pvec[:, 96 + s] = 1.0
        pvec[:, 100:108] = f("b_gate")[0][None, :]
        pvec[:, 108:144] = brt[None, :]
        sm = f("state_mlstm_m")[0, sb].reshape(16)
        pvec[:, 144:160] = sm[None, :]
        in_maps.append(dict(
            xTl=np.ascontiguousarray(xp[b].T.reshape(KC, 128, 8, 512).transpose(2, 1, 0, 3)).reshape(8, 128, KC * 512),
            xTs=np.ascontiguousarray(xs_flat.T.reshape(KC, 128, 32).transpose(1, 0, 2)).reshape(128, KC * 32),
            cvl=np.ascontiguousarray(f("cache_win_v")[0, sb].reshape(4, 16, 128, 8, 128).transpose(0, 3, 2, 1, 4)).reshape(4, 8, 128, 2048),
            xown=np.ascontiguousarray(np.concatenate([xp[b, 1024 * s:1024 * (s + 1)], xs_flat], axis=0)),
            w_in_l=w_in_l, wgate8=wgate8, w_out=w_out, pvec=pvec, srow=sm[None, :].copy(), lnp=lnp, wr=wr, wg=wg, wu=wu, wd=wd,
            sconvT=np.ascontiguousarray(f("state_conv")[0, sb].transpose(0, 2, 1)),
            sC=np.ascontiguousarray(f("state_mlstm_C")[0, sb]), sn=np.ascontiguousarray(f("state_mlstm_n")[0, sb]),
            ckT=np.ascontiguousarray(f("cache_win_k")[0, sb].transpose(0, 2, 3, 1)),
            ck=np.ascontiguousarray(f("cache_win_k")[0, sb]), cv=np.ascontiguousarray(f("cache_win_v")[0, sb]),
            cst=cst, bt=bt, bs=bs))
    res = run_bass_kernel_spmd(nc, in_maps, core_ids=list(range(ncores))).results
    _CACHE["res"] = res
    if ncores < N_CORES:
        return res
    y_p = np.zeros((2, T, D), np.float32)
    y_s = np.zeros((32, 8, D), np.float32)
    for c in range(8):
        b, s = c // 4, c % 4
        y_p[b, 1024 * s:1024 * (s + 1)] = res[c]["y"][0:1024]
        y_s[4 * c:4 * c + 4] = res[c]["y"][1024:1056].reshape(4, 8, D)
    lead = [res[0], res[4]]
    p_conv = np.stack([r["pconvT"].T for r in lead])[None]
    p_C = np.stack([r["pC"] for r in lead])[None]
    p_n = np.stack([r["pn"] for r in lead])[None]
    p_m = np.stack([r["pm"][0] for r in lead])[None]
    p_wk = np.stack([r["pwkT"].transpose(2, 0, 1) for r in lead])[None]
    p_wv = np.stack([r["pwv"] for r in lead])[None]
    s_conv = np.concatenate([r["oconvT"].transpose(0, 2, 1) for r in res])[None]
    s_C = np.concatenate([r["oC"] for r in res])[None]
    s_n = np.concatenate([r["on"] for r in res])[None]
    s_m = np.concatenate([r["om"].reshape(4, 4) for r in res])[None]
    s_wk = np.concatenate([r["owk"] for r in res])[None]
    s_wv = np.concatenate([r["owv"] for r in res])[None]
    outs = (y_p, y_s, p_conv, p_C, p_n, p_m, p_wk, p_wv, s_conv, s_C, s_n, s_m, s_wk, s_wv)
    return tuple(np.ascontiguousarray(o, dtype=np.float32) for o in outs)
```

```python
import math
import numpy as np
import concourse.bass as bass
import concourse.mybir as mybir
from concourse.bass_utils import run_bass_kernel_spmd

F32 = mybir.dt.float32
BF16 = mybir.dt.bfloat16
AF = mybir.ActivationFunctionType
ALU = mybir.AluOpType
AX = mybir.AxisListType

ENGS = ("pe", "act", "dve", "pool", "sp")
N_CORES = 8
E_RUN = 32
STOP = 99


class _Stop(Exception):
    pass
T = 4096
TS = 32
TT = T + TS
NO = 1056
D = 2048
KC = 16
NCOL = 7176
ALPHA = 2.0 ** 0.25
EPS = 1e-5
NEG = -30000.0
ARENA = 53200


class Prog:
    def __init__(self, nc):
        self.nc = nc
        self.ops = []
        self.frozen = False

    def add(self, eng, fn, reads=(), writes=(), dma_sem=None):
        if self.frozen:
            return
        writes = tuple(writes) + tuple(k for k in reads if isinstance(k, tuple) and k[0] == "ps" and k not in writes)
        self.ops.append(dict(eng=eng, fn=fn, reads=tuple(reads), writes=tuple(writes), dma_sem=dma_sem))

    def pe(self, fn, reads=(), writes=()):
        self.add("pe", fn, reads, writes)

    def act(self, fn, reads=(), writes=()):
        self.add("act", fn, reads, writes)

    def dve(self, fn, reads=(), writes=()):
        self.add("dve", fn, reads, writes)

    def dma(self, eng, out, in_, reads=(), writes=(), sem=None, **kw):
        if sem is None:
            sem = ("d",) + tuple(writes[:1] or reads[:1])
        self.add(eng, lambda e: e.dma_start(out=out, in_=in_, **kw), reads, writes, dma_sem=sem)

    def barrier(self):
        if self.frozen:
            return
        self.ops.append(None)

    def build(self):
        nc = self.nc
        segs = [[]]
        for op in self.ops:
            if op is None:
                segs.append([])
            else:
                segs[-1].append(op)
        sems = {}
        sem_ctx = []

        def get_sem(name):
            if name not in sems:
                g = nc.semaphore("s%d" % len(sems))
                sems[name] = g.__enter__()
                sem_ctx.append(g)
            return sems[name]

        eng_cnt = {e: 0 for e in ENGS}
        dma_cnt = {}
        wd = {e: {} for e in ENGS}
        self.n_waits = 0
        for si_, seg in enumerate(segs):
            is_last = si_ == len(segs) - 1
            ops = seg
            n = len(ops)
            last_writer, readers = {}, {}
            deps = [None] * n
            eng_seq = {e: 0 for e in ENGS}
            seq_of = [0] * n
            waited = {c: {p: -1 for p in ENGS} for c in ENGS}
            signaled = [False] * n
            for i, op in enumerate(ops):
                e = op["eng"]
                seq_of[i] = eng_seq[e]
                eng_seq[e] += 1
                cand = set()
                for k in op["reads"]:
                    w = last_writer.get(k)
                    if w is not None:
                        cand.add((w, True))
                for k in op["writes"]:
                    w = last_writer.get(k)
                    if w is not None:
                        cand.add((w, False))
                    for r in readers.get(k, {}).values():
                        cand.add((r, False))
                dd, best = [], {}
                for (j, raw) in cand:
                    pj = ops[j]
                    if j == i:
                        continue
                    if pj["dma_sem"] is not None:
                        dd.append(j)
                        continue
                    if pj["eng"] == e and not raw:
                        continue
                    if seq_of[j] <= waited[e][pj["eng"]]:
                        continue
                    pe_ = pj["eng"]
                    if pe_ not in best or seq_of[j] > seq_of[best[pe_]]:
                        best[pe_] = j
                for pe_, j in best.items():
                    waited[e][pe_] = seq_of[j]
                    dd.append(j)
                    signaled[j] = True
                deps[i] = sorted(set(dd))
                rk = e if op["dma_sem"] is None else ("dma", i)
                for k in op["reads"]:
                    readers.setdefault(k, {})[rk] = i
                for k in op["writes"]:
                    last_writer[k] = i
                    readers[k] = {}
            sig_val = [None] * n
            for i, op in enumerate(ops):
                if op["dma_sem"] is not None:
                    s = op["dma_sem"]
                    dma_cnt[s] = dma_cnt.get(s, 0) + 16
                    sig_val[i] = (("dma", s), dma_cnt[s])
                elif signaled[i]:
                    eng_cnt[op["eng"]] += 1
                    sig_val[i] = (("eng", op["eng"]), eng_cnt[op["eng"]])
            for i in range(n):
                if sig_val[i] is not None:
                    get_sem(sig_val[i][0])
            per_eng = {e: [i for i in range(n) if ops[i]["eng"] == e] for e in ENGS}
            dma_snapshot = dict(dma_cnt)

            def emit_engine(ename):
                def body(eng):
                    for i in per_eng[ename]:
                        op = ops[i]
                        for j in deps[i]:
                            key, val = sig_val[j]
                            if wd[ename].get(key, 0) >= val:
                                continue
                            wd[ename][key] = val
                            eng.wait_ge(get_sem(key), val)
                            self.n_waits += 1
                        ins = op["fn"](eng)
                        if sig_val[i] is not None:
                            key, val = sig_val[i]
                            ins.then_inc(get_sem(key), 16 if key[0] == "dma" else 1)
                    if ename == "sp":
                        for s, v in dma_snapshot.items():
                            if s == "shift" and not is_last:
                                continue
                            if wd["sp"].get(("dma", s), 0) < v:
                                wd["sp"][("dma", s)] = v
                                eng.wait_ge(get_sem(("dma", s)), v)
                return body

            with nc.Block() as block:
                block.tensor(emit_engine("pe"))
                block.scalar(emit_engine("act"))
                block.vector(emit_engine("dve"))
                block.gpsimd(emit_engine("pool"))
                block.sync(emit_engine("sp"))
        for g in reversed(sem_ctx):
            g.__exit__(None, None, None)


def build_program():
    nc = bass.Bass("TRN2", target_bir_lowering=False)
    P = Prog(nc)

    def din(name, shape):
        return nc.dram_tensor(name, list(shape), F32, kind="ExternalInput").ap()

    def dout(name, shape):
        return nc.dram_tensor(name, list(shape), F32, kind="ExternalOutput").ap()

    xTl_d = din("xTl", [8, 128, KC * 512])
    xTs_d = din("xTs", [128, KC * 32])
    xown_d = din("xown", [NO, D])
    winl_d = din("w_in_l", [56, 128, KC * 128])
    wgt_d = din("wgate8", [D, 8])
    wout_d = din("w_out", [D, D])
    pvec_d = din("pvec", [128, 256])
    srow_d = din("srow", [1, 16])
    lnp_d = din("lnp", [4, 128, D])
    wr_d = din("wr", [D, 36])
    wg_d = din("wg", [32, 8, 128, KC * 128])
    wu_d = din("wu", [32, 8, 128, KC * 128])
    wd_d = din("wd", [32, 1024, D])
    sconv_d = din("sconvT", [4, D, 3])
    sC_d = din("sC", [4, 4, 256, 256])
    sn_d = din("sn", [4, 4, 256])
    ckT_d = din("ckT", [4, 8, 128, 2048])
    ck_d = din("ck", [4, 2048, 8, 128])
    cv_d = din("cv", [4, 2048, 8, 128])
    cvl_d = din("cvl", [4, 8, 128, 16 * 128])
    cst_d = din("cst", [128, 512])
    bt_d = din("bt", [8, 128, 23 * 128])
    bs_d = din("bs", [8, 128, 136])

    y_d = dout("y", [NO, D])
    pconv_d = dout("pconvT", [D, 3])
    pC_d = dout("pC", [4, 256, 256])
    pn_d = dout("pn", [4, 256])
    pm_d = dout("pm", [1, 4])
    pwk_d = dout("pwkT", [8, 128, 2048])
    pwv_d = dout("pwv", [2048, 8, 128])
    oconv_d = dout("oconvT", [4, D, 3])
    oC_d = dout("oC", [4, 4, 256, 256])
    on_d = dout("on", [4, 4, 256])
    om_d = dout("om", [1, 16])
    owk_d = dout("owk", [4, 2048, 8, 128])
    owv_d = dout("owv", [4, 2048, 8, 128])

    with nc.allow_non_contiguous_dma(reason="tiny strided state vectors"), \
            nc.sbuf_tensor("arena", [128, ARENA], F32) as arena:
        pst = []
        pctx = []
        for i in range(8):
            g = nc.psum_tensor("ps%d" % i, [128, 512], F32)
            pst.append(g.__enter__())
            pctx.append(g)
        ps = [t[:] for t in pst]
        psb = [t[:].bitcast(BF16) for t in pst]
        A = arena

        def fv(off, n):
            return A[:, off:off + n]

        def bv(off, n):
            return A[:, off:off + n // 2].bitcast(BF16)

        def v3(ap, b):
            return ap.rearrange("p (a b) -> p a b", b=b)

        class Al:
            def __init__(self, start):
                self.o = start

            def f(self, n):
                o = self.o
                self.o += n
                assert self.o <= ARENA, self.o
                return fv(o, n)

            def b(self, n):
                n2 = (n + 1) // 2 * 2
                o = self.o
                self.o += n2 // 2
                assert self.o <= ARENA, self.o
                return bv(o, n2)[:, 0:n]

        def MM(out, lhsT, rhs, start, stop, rd, wr):
            P.pe(lambda e: e.matmul(out, lhsT=lhsT, rhs=rhs, start=start, stop=stop), rd, wr)

        def TR(out, in_, ident, rd, wr):
            P.pe(lambda e: e.transpose(out, in_, ident), rd, wr)

        def ACT(out, in_, func, rd, wr, **kw):
            P.act(lambda e: e.activation(out=out, in_=in_, func=func, **kw), rd, wr)

        def TS(out, in0, s1, s2, op0, op1, rd, wr):
            if op1 is None:
                P.dve(lambda e: e.tensor_scalar(out=out, in0=in0, scalar1=s1, scalar2=None, op0=op0), rd, wr)
            else:
                P.dve(lambda e: e.tensor_scalar(out=out, in0=in0, scalar1=s1, scalar2=s2, op0=op0, op1=op1), rd, wr)

        def STT(out, in0, scalar, in1, op0, op1, rd, wr):
            P.dve(lambda e: e.scalar_tensor_tensor(out=out, in0=in0, scalar=scalar, in1=in1, op0=op0, op1=op1), rd, wr)

        def TTo(out, in0, in1, op, rd, wr):
            P.dve(lambda e: e.tensor_tensor(out=out, in0=in0, in1=in1, op=op), rd, wr)

        def CP(out, in_, rd, wr):
            P.dve(lambda e: e.tensor_copy(out=out, in_=in_), rd, wr)

        def MS(ap, val, wr):
            P.dve(lambda e: e.memset(ap, val), (), wr)

        def RMAX(out, in_, rd, wr):
            P.dve(lambda e: e.reduce_max(out=out, in_=in_, axis=AX.X), rd, wr)

        def RCP(out, in_, rd, wr):
            P.dve(lambda e: e.reciprocal(out=out, in_=in_), rd, wr)

        pa = Al(0)
        cst = pa.f(512)
        identF = cst[:, 0:128]
        onesF = cst[:, 128:256]
        tri = cst[:, 256:320]
        identB = pa.b(128)
        pvec = pa.f(256)
        cw = v3(pvec[:, 0:64], 4)
        cb = pvec[:, 64:80]
        gain = pvec[:, 80:96]
        sel = pvec[:, 96:100]
        bg = pvec[:, 100:108]
        brt = pvec[:, 108:144]
        smr = pvec[:, 144:160]
        nbg = pa.f(8)
        SG = pa.f(64)
        SG3 = v3(SG, 4)
        esm = pa.f(16)
        wg8 = pa.b(KC * 8)
        wg83 = v3(wg8, 8)
        srow = pa.f(16)
        TSETS = []
        for _c in range(4):
            TSETS.append(dict(c=_c, junk=pa.b(256), s1=pa.f(1), s2=pa.f(1), dm=pa.f(1), t0=pa.f(1), mean=pa.f(1),
                              t1a=pa.f(1), nt1=pa.f(1), sd=pa.f(1), rstd=pa.f(1), ha=pa.b(128)))

        def run_interleaved(gens, stagger=0):
            pending = list(gens)
            active = []
            rnd = 0
            while pending or active:
                if pending and (stagger == 0 or rnd % stagger == 0):
                    if stagger == 0:
                        active.extend(pending)
                        pending = []
                    else:
                        active.append(pending.pop(0))
                for g in list(active):
                    try:
                        next(g)
                    except StopIteration:
                        active.remove(g)
                rnd += 1
        CAT_OFF = pa.o
        catT = pa.b(KC * NO)
        catT3 = v3(catT, NO)
        P_END = pa.o
        ACC0 = ARENA - (9 * D + KC * NO // 2 + 9 * 32)
        accA = Al(ACC0)
        acc = accA.f(9 * D)
        acc3 = v3(acc, D)
        x1Tb = accA.b(KC * NO)
        x1Tb3 = v3(x1Tb, NO)
        gates = accA.f(9 * 32)
        gates3 = v3(gates, 32)

        def wblk(cb):
            return winl_d[cb].rearrange("p (kc n) -> p kc n", n=128)

        def xblk(blk):
            if blk < 8:
                return xTl_d[blk].rearrange("p (kc t) -> p kc t", t=512)
            return xTs_d.rearrange("p (kc t) -> p kc t", t=32)

        P.dma("sp", cst, cst_d, writes=["cst"])
        P.dma("sp", pvec, pvec_d, writes=["pvec"])
        P.dma("sp", srow[0:1, :], srow_d, writes=["srow"])
        P.dma("pool", wg83, wgt_d.rearrange("(kc p) n -> p kc n", p=128), writes=["wg8"], sem="setup2")
        for b in range(4):
            P.dma("sp", owk_d[b, 0:2040], ck_d[b, 8:2048], writes=[("owk", b)], sem="shift")
            P.dma("sp", owv_d[b, 0:2040], cv_d[b, 8:2048], writes=[("owv", b)], sem="shift")
        CP(identB, identF, ["cst"], ["identB"])
        TS(nbg, bg, -1.0, None, ALU.mult, None, ["pvec"], ["nbg"])
        for s_ in range(4):
            TS(SG3[:, :, s_], gain, sel[:, s_:s_ + 1], None, ALU.mult, None, ["pvec"], ["SG"])
        ACT(esm, smr, AF.Exp, ["pvec"], ["esm"])
        MS(catT, 0.0, [("catT", i) for i in range(16)])
        P.barrier()
        if STOP == 0:
            P.frozen = True

        a1 = Al(P_END)
        Wm = a1.b(KC * 1024)
        Wm3 = v3(Wm, 1024)
        xb = [v3(a1.b(KC * 512), 512) for _ in range(2)]
        U = [a1.f(515) for _ in range(4)]
        Us = [v3(a1.f(44), 11) for _ in range(4)]
        cacc = a1.f(512)
        sgt = a1.f(512)
        QT = [a1.b(512) for _ in range(2)]
        KT = [a1.b(512) for _ in range(2)]
        sigO = v3(a1.b(8 * 256), 256)
        Vf = v3(a1.f(8 * 257), 257)
        G = v3(a1.f(64), 8)
        gz = a1.f(8)
        gsp = a1.f(8)
        gr = a1.f(8)
        ger = a1.f(8)
        genb = a1.f(8)
        geB = a1.f(8)
        gerr = a1.f(8)
        grm = a1.f(1)
        Vp = v3(a1.b(8 * 258), 258)
        Vpp = v3(a1.b(8 * 258), 258)
        Ktok = v3(a1.b(8 * 256), 256)
        PmT = a1.b(64)
        Cf = a1.f(514)
        Cf3 = v3(Cf, 257)
        Cb = a1.b(514)
        Cb3 = v3(Cb, 257)
        Cfs = [a1.f(514) for _ in range(4)]
        Cbs = [a1.b(514) for _ in range(4)]
        hnS = [a1.f(256) for _ in range(4)]
        hmS = [a1.b(256) for _ in range(4)]
        numS = [a1.f(258) for _ in range(4)]
        PmTs = [a1.b(64) for _ in range(2)]
        Rrow = a1.f(72)
        Brow = a1.f(72)
        mrow = a1.f(8)
        enm = a1.f(8)
        enmb = a1.f(8)
        Cout = a1.f(514)
        Cout3 = v3(Cout, 257)

        def norm_gen(L, num, W, center, dm_fn, out_fn, tag, TS_):
            c = TS_["c"]
            k = lambda n: (n, c)
            R = slice(0, L)
            ACT(TS_["junk"][R, 0:W], num, AF.Copy, [tag], [k("junk"), k("s1")], accum_out=TS_["s1"][R, :])
            yield
            ACT(TS_["junk"][R, 0:W], num, AF.Square, [tag], [k("junk"), k("s2")], accum_out=TS_["s2"][R, :])
            yield
            dm_fn()
            yield
            TS(TS_["t0"][R, :], TS_["dm"][R, :], TS_["dm"][R, :], EPS, ALU.mult, ALU.mult, [k("dm")], [k("t0")])
            if center:
                TS(TS_["mean"][R, :], TS_["s1"][R, :], 1.0 / W, None, ALU.mult, None, [k("s1")], [k("mean")])
            else:
                MS(TS_["mean"][R, :], 0.0, [k("mean")])
            yield
            STT(TS_["t1a"][R, :], TS_["s2"][R, :], 1.0 / W, TS_["t0"][R, :], ALU.mult, ALU.add, [k("s2"), k("t0")], [k("t1a")])
            yield
            STT(TS_["nt1"][R, :], TS_["mean"][R, :], TS_["mean"][R, :], TS_["t1a"][R, :], ALU.mult, ALU.subtract,
                [k("mean"), k("t1a")], [k("nt1")])
            yield
            ACT(TS_["sd"][R, :], TS_["nt1"][R, :], AF.Sqrt, [k("nt1")], [k("sd")], scale=-1.0)
            yield
            RCP(TS_["rstd"][R, :], TS_["sd"][R, :], [k("sd")], [k("rstd")])
            yield
            yield from out_fn()

        MS(Vf[:, :, 256:257], 1.0, ["Vf1"])
        for h in range(4):
            for j_, c0 in enumerate((2 * h, 8 + 2 * h, 24 + 2 * h, 16 + 2 * h)):
                for i_ in range(2):
                    P.dma("pool", Wm3[:, :, j_ * 256 + i_ * 128:j_ * 256 + (i_ + 1) * 128], wblk(c0 + i_),
                          writes=["Wm"], sem="Wm")
            for cc in range(4):
                MS(U[cc][:, 0:3], 0.0, [("U", cc)])
            MS(Cf, 0.0, ["Cf"])
            MS(Cb, 0.0, ["Cb"])
            for blk in range(9):
                samp = blk == 8
                N = 32 if samp else 512
                L = 8 if samp else 64
                nch = 4 if samp else 8
                xs = blk % 2
                xk = ("xb", xs)
                P.dma("pool", xb[xs][:, :, 0:N], xblk(blk), writes=[xk], sem=xk)
                for cc in range(4):
                    gcc = 2 * h + cc if cc < 2 else 8 + 2 * h + (cc - 2)
                    pb = cc % 2
                    for kc in range(KC):
                        MM(ps[pb][:, 0:N], Wm3[:, kc, cc * 128:(cc + 1) * 128], xb[xs][:, kc, 0:N],
                           kc == 0, kc == KC - 1, ["Wm", xk], [("ps", pb)])
                    uk = ("U", cc)
                    if not samp:
                        ACT(U[cc][:, 3:3 + N], ps[pb][:, 0:N], AF.Copy, [("ps", pb)], [uk])
                        src = lambda j: U[cc][:, j:j + N]
                        accv = cacc[:, 0:N]
                        sgv = sgt[:, 0:N]
                    else:
                        uk = ("Us", cc)
                        for b in range(4):
                            P.dma("sp", Us[cc][:, b, 0:3], sconv_d[b, gcc * 128:(gcc + 1) * 128, :],
                                  writes=[uk], sem=("Usl", cc))
                        ACT(Us[cc][:, :, 3:11], ps[pb][:, 0:32].rearrange("p (b t) -> p b t", t=8),
                            AF.Copy, [("ps", pb)], [uk])
                        src = lambda j: Us[cc][:, :, j:j + 8]
                        accv = cacc[:, 0:32].rearrange("p (b t) -> p b t", t=8)
                        sgv = sgt[:, 0:32].rearrange("p (b t) -> p b t", t=8)
                    TS(accv, src(3), cw[:, gcc, 3:4], cb[:, gcc:gcc + 1], ALU.mult, ALU.add, [uk, "pvec"], ["cacc"])
                    for j in (2, 1, 0):
                        STT(accv, src(j), cw[:, gcc, j:j + 1], accv, ALU.mult, ALU.add, [uk, "pvec", "cacc"], ["cacc"])
                    ACT(sgv, accv, AF.Sigmoid, ["cacc"], ["sgt"])
                    dst = QT[cc] if cc < 2 else KT[cc - 2]
                    dk = ("QT", cc) if cc < 2 else ("KT", cc - 2)
                    dstv = dst[:, 0:N] if not samp else dst[:, 0:32].rearrange("p (b t) -> p b t", t=8)
                    STT(dstv, accv, 1.0 if cc < 2 else 0.0625, sgv, ALU.mult, ALU.mult, ["cacc", "sgt"], [dk])
                    if not samp:
                        if blk == 7:
                            P.dma("sp", pconv_d[gcc * 128:(gcc + 1) * 128, :], U[cc][:, 512:515],
                                  reads=[uk], writes=[("pconv", gcc)], sem=("pco", cc))
                        CP(U[cc][:, 0:3], U[cc][:, N:N + 3], [uk], [uk])
                    else:
                        for b in range(4):
                            P.dma("sp", oconv_d[b, gcc * 128:(gcc + 1) * 128, :], Us[cc][:, b, 8:11],
                                  reads=[uk], writes=[("oconv", gcc, b)], sem=("oco", cc))
                for j in range(nch):
                    lt = lambda kc: xb[xs][:, kc, j * L:(j + 1) * L]
                    po, pv_ = 2 + j % 2, 4 + j % 2
                    for kc in range(KC):
                        MM(ps[po][0:L, 0:256], lt(kc), Wm3[:, kc, 512:768], kc == 0, kc == KC - 1,
                           ["Wm", xk], [("ps", po)])
                    ACT(sigO[0:L, j, :], ps[po][0:L, 0:256], AF.Sigmoid, [("ps", po)], [("sigO", j)])
                    for kc in range(KC):
                        MM(ps[pv_][0:L, 0:256], lt(kc), Wm3[:, kc, 768:1024], kc == 0, kc == KC - 1,
                           ["Wm", xk], [("ps", pv_)])
                    ACT(Vf[0:L, j, 0:256], ps[pv_][0:L, 0:256], AF.Copy, [("ps", pv_)], [("Vf", j)])
                    for kc in range(KC):
                        MM(ps[6][0:L, j * 8:(j + 1) * 8], lt(kc), wg83[:, kc, :], kc == 0, kc == KC - 1,
                           ["wg8", xk], [("ps", 6)])
                CP(G[0:L, 0:nch, :], ps[6][0:L, 0:nch * 8].rearrange("p (a b) -> p a b", b=8), [("ps", 6)], ["G"])
                igv = G[0:L, 0:nch, h]
                fgv = G[0:L, 0:nch, 4 + h]
                ACT(gz[0:L, 0:nch], fgv, AF.Exp, ["G", "nbg"], ["gz"], scale=-1.0, bias=nbg[0:L, 4 + h:5 + h])
                ACT(gsp[0:L, 0:nch], gz[0:L, 0:nch], AF.Ln, ["gz"], ["gsp"], bias=1.0)
                MM(ps[7][0:L, 0:nch], tri[0:L, 0:L], gsp[0:L, 0:nch], True, True, ["cst", "gsp"], [("ps", 7)])
                MM(ps[7][:, 16:16 + nch], onesF[0:L, :], gsp[0:L, 0:nch], True, True, ["cst", "gsp"], [("ps", 7)])
                STT(gr[0:L, 0:nch], igv, bg[0:L, h:h + 1], ps[7][0:L, 0:nch], ALU.add, ALU.add,
                    ["G", "pvec", ("ps", 7)], ["gr"])
                ACT(ger[0:L, 0:nch], gr[0:L, 0:nch], AF.Exp, ["gr"], ["ger"])
                ACT(genb[0:L, 0:nch], ps[7][0:L, 0:nch], AF.Exp, [("ps", 7)], ["genb"])
                ACT(geB[:, 0:nch], ps[7][:, 16:16 + nch], AF.Exp, [("ps", 7)], ["geB"], scale=-1.0)
                TTo(gerr[0:L, 0:nch], ger[0:L, 0:nch], geB[0:L, 0:nch], ALU.mult, ["ger", "geB"], ["gerr"])
                TS(Brow[0:1, blk * 8:blk * 8 + nch], ps[7][0:1, 16:16 + nch], -1.0, None, ALU.mult, None,
                   [("ps", 7)], ["Brow"])
                TR(ps[7][0:nch, 32:32 + L], gr[0:L, 0:nch], identF[0:L, 0:L], ["gr", "cst"], [("ps", 7)])
                RMAX(grm[0:nch, :], ps[7][0:nch, 32:32 + L], [("ps", 7)], ["grm"])
                TR(ps[7][0:1, 112:112 + nch], grm[0:nch, :], identF[0:nch, 0:nch], ["grm", "cst"], [("ps", 7)])
                CP(Rrow[0:1, blk * 8:blk * 8 + nch], ps[7][0:1, 112:112 + nch], [("ps", 7)], ["Rrow"])
                for j in range(nch):
                    if blk == 0 and h == 0:
                        pass
                    TS(Vp[0:L, j, 0:257], Vf[0:L, j, :], ger[0:L, j:j + 1], None, ALU.mult, None,
                       [("Vf", j), "Vf1", "ger"], [("Vp", j)])
                    ACT(Vpp[0:L, j, 0:257], Vf[0:L, j, :], AF.Copy, [("Vf", j), "Vf1", "gerr"], [("Vpp", j)],
                        scale=gerr[0:L, j:j + 1])
                    pk = 4 + j % 2
                    for ec in range(2):
                        TR(psb[pk][0:L, ec * 128:(ec + 1) * 128], KT[ec][:, j * L:(j + 1) * L], identB,
                           [("KT", ec), "identB"], [("ps", pk)])
                    CP(Ktok[0:L, j, :], psb[pk][0:L, 0:256], [("ps", pk)], [("Ktok", j)])
                if samp:
                    for b in range(4):
                        Cs3 = v3(Cfs[b], 257)
                        P.dma("sp", Cs3[:, :, 0:256], sC_d[b, h].rearrange("(ec p) f -> p ec f", p=128),
                              writes=[("Cfs", b)], sem=("Cfl", b))
                        P.dma("sp", Cs3[:, :, 256:257], sn_d[b, h].rearrange("(ec p one) -> p ec one", p=128, one=1),
                              writes=[("Cfs", b)], sem=("Cfl", b))
                        TS(Cfs[b], Cfs[b], esm[:, b * 4 + h:b * 4 + h + 1], None, ALU.mult, None,
                           [("Cfs", b), "esm"], [("Cfs", b)])
                        ACT(Cbs[b], Cfs[b], AF.Copy, [("Cfs", b)], [("Cbs", b)])
                def chunk_gen(j, L=L, samp=samp, blk=blk, h=h):
                    c = j % 4
                    T_ = TSETS[c]
                    k = lambda n: (n, c)
                    if samp:
                        cf, cbv, cfk, cbk = Cfs[j], Cbs[j], ("Cfs", j), ("Cbs", j)
                    else:
                        cf, cbv, cfk, cbk = Cf, Cb, "Cf", "Cb"
                    cf3, cb3 = v3(cf, 257), v3(cbv, 257)
                    qs = [QT[ec][:, j * L:(j + 1) * L] for ec in range(2)]
                    ks = [KT[ec][:, j * L:(j + 1) * L] for ec in range(2)]
                    pm = PmTs[j % 2]
                    pmk = ("PmT", j % 2)
                    for ec in range(2):
                        MM(ps[0][0:L, 0:L], ks[ec], qs[ec], ec == 0, ec == 1, [("KT", ec), ("QT", ec)], [("ps", 0)])
                    yield
                    TTo(pm[0:L, 0:L], ps[0][0:L, 0:L], tri[0:L, 0:L], ALU.mult, [("ps", 0), "cst"], [pmk])
                    yield
                    MM(ps[1][0:L, 0:257], pm[0:L, 0:L], Vp[0:L, j, 0:257], True, False, [pmk, ("Vp", j)], [("ps", 1)])
                    for ec in range(2):
                        MM(ps[1][0:L, 0:257], qs[ec], cb3[:, ec, :], False, ec == 1, [("QT", ec), cbk], [("ps", 1)])
                    yield
                    for ec in range(2):
                        MM(ps[2 + ec][:, 0:257], Ktok[0:L, j, ec * 128:(ec + 1) * 128], Vpp[0:L, j, 0:257],
                           True, True, [("Ktok", j), ("Vpp", j)], [("ps", 2 + ec)])
                        STT(cf3[:, ec, :], cf3[:, ec, :], geB[:, j:j + 1], ps[2 + ec][:, 0:257], ALU.mult, ALU.add,
                            [cfk, "geB", ("ps", 2 + ec)], [cfk])
                    yield
                    CP(cbv, cf, [cfk], [cbk])
                    ACT(numS[c][0:L, 0:257], ps[1][0:L, 0:257], AF.Copy, [("ps", 1)], [k("numS")])
                    yield

                    def dmf():
                        ACT(T_["t0"][0:L, :], numS[c][0:L, 256:257], AF.Abs, [k("numS")], [k("t0")])
                        TS(T_["dm"][0:L, :], T_["t0"][0:L, :], genb[0:L, j:j + 1], None, ALU.max, None, [k("t0"), "genb"], [k("dm")])

                    def outf():
                        TS(hnS[c][0:L, :], numS[c][0:L, 0:256], T_["mean"][0:L, :], T_["rstd"][0:L, :], ALU.subtract, ALU.mult,
                           [k("numS"), k("mean"), k("rstd")], [k("hn")])
                        yield
                        TTo(hmS[c][0:L, :], hnS[c][0:L, :], sigO[0:L, j, :], ALU.mult, [k("hn"), ("sigO", j)], [k("hm")])
                        yield
                        for fh in range(2):
                            pc = (c * 2 + fh) * 64
                            TR(psb[6][:, pc:pc + L], hmS[c][0:L, fh * 128:(fh + 1) * 128], identB[0:L, 0:L],
                               [k("hm"), "identB"], [("ps", 6)])
                        yield
                        for fh in range(2):
                            pc = (c * 2 + fh) * 64
                            fc = 2 * h + fh
                            if not samp:
                                col = (blk % 2) * 512 + j * 64
                                STT(catT3[:, fc, col:col + 64], psb[6][:, pc:pc + 64], SG3[:, fc, blk // 2:blk // 2 + 1],
                                    catT3[:, fc, col:col + 64], ALU.mult, ALU.add, [("ps", 6), "SG", ("catT", fc)], [("catT", fc)])
                            else:
                                col = 1024 + 8 * j
                                TS(catT3[:, fc, col:col + 8], psb[6][:, pc:pc + 8], gain[:, fc:fc + 1], None,
                                   ALU.mult, None, [("ps", 6), "pvec"], [("catT", fc)])
                        yield

                    yield from norm_gen(L, numS[c][0:L, 0:256], 256, True, dmf, outf, k("numS"), T_)

                run_interleaved([chunk_gen(j) for j in range(nch)], stagger=3)
            MS(mrow[0:1, 0:1], 0.0, ["mrow"])
            for c in range(64):
                STT(mrow[0:1, 0:1], mrow[0:1, 0:1], Rrow[0:1, c:c + 1], Brow[0:1, c:c + 1], ALU.max, ALU.add,
                    ["mrow", "Rrow", "Brow"], ["mrow"])
            TTo(mrow[0:1, 4:8], srow[0:1, :].rearrange("p (b h) -> p b h", h=4)[:, :, h], Rrow[0:1, 64:68], ALU.max,
                ["srow", "Rrow"], ["mrow2"])
            TTo(mrow[0:1, 4:8], mrow[0:1, 4:8], Brow[0:1, 64:68], ALU.add, ["mrow2", "Brow"], ["mrow2"])
            P.dma("sp", pm_d[0:1, h:h + 1], mrow[0:1, 0:1], reads=["mrow"], writes=[("pm", h)], sem="pmo1")
            P.dma("sp", om_d[0:1, :].rearrange("p (b h) -> p b h", h=4)[:, :, h], mrow[0:1, 4:8],
                  reads=["mrow2"], writes=[("om", h)], sem="pmo2")
            ACT(enm[0:1, 0:1], mrow[0:1, 0:1], AF.Exp, ["mrow"], ["enm"], scale=-1.0)
            ACT(enm[0:1, 4:8], mrow[0:1, 4:8], AF.Exp, ["mrow2"], ["enm"], scale=-1.0)
            MM(ps[7][:, 0:8], onesF[0:1, :], enm[0:1, 0:8], True, True, ["cst", "enm"], [("ps", 7)])
            CP(enmb[:, 0:8], ps[7][:, 0:8], [("ps", 7)], ["enmb"])
            TS(Cout, Cf, enmb[:, 0:1], None, ALU.mult, None, ["Cf", "enmb"], ["Cout"])
            P.dma("sp", pC_d[h].rearrange("(ec p) f -> p ec f", p=128), Cout3[:, :, 0:256],
                  reads=["Cout"], writes=[("pC", h)], sem="Co")
            P.dma("sp", pn_d[h].rearrange("(ec p one) -> p ec one", p=128, one=1), Cout3[:, :, 256:257],
                  reads=["Cout"], writes=[("pn", h)], sem="Co")
            for b in range(4):
                TS(Cout, Cfs[b], enmb[:, 4 + b:5 + b], None, ALU.mult, None, [("Cfs", b), "enmb"], ["Cout"])
                P.dma("sp", oC_d[b, h].rearrange("(ec p) f -> p ec f", p=128), Cout3[:, :, 0:256],
                      reads=["Cout"], writes=[("oC", h, b)], sem="Co")
                P.dma("sp", on_d[b, h].rearrange("(ec p one) -> p ec one", p=128, one=1), Cout3[:, :, 256:257],
                      reads=["Cout"], writes=[("on", h, b)], sem="Co")
        P.barrier()
        if STOP == 1:
            P.frozen = True

        a2 = Al(P_END)
        Wa3 = v3(a2.b(KC * 384), 384)
        xb2 = [v3(a2.b(KC * 512), 512) for _ in range(2)]
        KTa = a2.b(TT)
        QTa = a2.b(TT)
        Va = v3(a2.b(32 * 130), 130)
        Vs = v3(a2.b(4 * 130), 130)
        BT = a2.f(23 * 128)
        BS = a2.f(136)
        stg = [a2.f(512) for _ in range(2)]
        stv = [a2.f(128) for _ in range(2)]
        stk = a2.f(128)
        tS = [a2.f(512) for _ in range(2)]
        PT = [a2.b(512) for _ in range(2)]
        kcT = [a2.b(2056) for _ in range(2)]
        vc = [v3(a2.b(16 * 130), 130) for _ in range(2)]
        inv_sqrt_e = 1.0 / math.sqrt(128.0)
        MS(Va[:, :, 128:129], 1.0, ["Va1"])
        MS(Vs[:, :, 128:129], 1.0, ["Vs1"])
        for s_ in range(2):
            MS(vc[s_][:, :, 128:129], 1.0, [("vc1", s_)])

        def att_norm(L, psn, out_cols, scal, is_sel, fc, tag, c):
            T_ = TSETS[c]
            k = lambda n: (n, c)

            def dmf():
                CP(T_["dm"][0:L, :], psn[0:L, 128:129], [tag], [k("dm")])

            def outf():
                ACT(T_["ha"][0:L, :], psn[0:L, 0:128], AF.Copy, [tag, k("rstd")], [k("ha")], scale=T_["rstd"][0:L, :])
                yield
                TR(psb[7][:, c * 128:c * 128 + L], T_["ha"][0:L, :], identB[0:L, 0:L], [k("ha"), "identB"], [("ps", 7)])
                yield
                if is_sel:
                    STT(out_cols, psb[7][:, c * 128:c * 128 + L], scal, out_cols, ALU.mult, ALU.add,
                        [("ps", 7), "SG", ("catT", fc)], [("catT", fc)])
                else:
                    TS(out_cols, psb[7][:, c * 128:c * 128 + L], scal, None, ALU.mult, None, [("ps", 7), "pvec"], [("catT", fc)])
                yield

            return norm_gen(L, psn[0:L, 0:128], 128, False, dmf, outf, tag, T_)

        ci = 0
        for a in range(8):
            for j_, c0 in enumerate((32 + a, 40 + a, 48 + a)):
                P.dma("pool", Wa3[:, :, j_ * 128:(j_ + 1) * 128], wblk(c0), writes=["Wa"], sem="Wa")
            P.dma("sp", BT, bt_d[a], writes=["BT"])
            P.dma("sp", BS, bs_d[a], writes=["BS"])
            fc = 8 + a
            for blk in range(9):
                if STOP == 19 + blk and a == 0:
                    P.frozen = True
                samp = blk == 8
                N = 32 if samp else 512
                xs = blk % 2
                xk = ("xb", xs)
                P.dma("pool", xb2[xs][:, :, 0:N], xblk(blk), writes=[xk], sem=xk)
                for kc in range(KC):
                    MM(ps[0][:, 0:N], Wa3[:, kc, 128:256], xb2[xs][:, kc, 0:N], kc == 0, kc == KC - 1,
                       ["Wa", xk], [("ps", 0)])
                ACT(KTa[:, blk * 512:blk * 512 + N], ps[0][:, 0:N], AF.Copy, [("ps", 0)], ["KTa"])
                if 4 <= blk < 8:
                    sg_ = blk % 2
                    CP(stg[sg_], ps[0][:, 0:512], [("ps", 0)], [("stg", sg_)])
                    P.dma("sp", pwk_d[a][:, (blk - 4) * 512:(blk - 3) * 512], stg[sg_], reads=[("stg", sg_)],
                          writes=[("pwk", a, blk)], sem=("stg", sg_))
                for kc in range(KC):
                    MM(ps[1][:, 0:N], Wa3[:, kc, 0:128], xb2[xs][:, kc, 0:N], kc == 0, kc == KC - 1,
                       ["Wa", xk], [("ps", 1)])
                ACT(QTa[:, blk * 512:blk * 512 + N], ps[1][:, 0:N], AF.Copy, [("ps", 1)], ["QTa"], scale=inv_sqrt_e)
                if not samp:
                    for tt in range(4):
                        pv_ = 2 + tt % 2
                        for kc in range(KC):
                            MM(ps[pv_][:, 0:128], xb2[xs][:, kc, tt * 128:(tt + 1) * 128], Wa3[:, kc, 256:384],
                               kc == 0, kc == KC - 1, ["Wa", xk], [("ps", pv_)])
                        ACT(Va[:, blk * 4 + tt, 0:128], ps[pv_][:, 0:128], AF.Copy, [("ps", pv_)], ["Va"])
                        if blk >= 4:
                            sv = tt % 2
                            CP(stv[sv], ps[pv_][:, 0:128], [("ps", pv_)], [("stv", sv)])
                            r0 = (blk - 4) * 512 + tt * 128
                            P.dma("sp", pwv_d[r0:r0 + 128, a, :], stv[sv], reads=[("stv", sv)],
                                  writes=[("pwv", a, blk, tt)], sem=("stv", sv))
                else:
                    for b in range(4):
                        for (wsl, which) in ((slice(256, 384), "v"), (slice(128, 256), "k")):
                            pv_ = 2 + b % 2
                            for kc in range(KC):
                                MM(ps[pv_][0:8, 0:128], xb2[xs][:, kc, b * 8:(b + 1) * 8], Wa3[:, kc, wsl],
                                   kc == 0, kc == KC - 1, ["Wa", xk], [("ps", pv_)])
                            if which == "v":
                                ACT(Vs[0:8, b, 0:128], ps[pv_][0:8, 0:128], AF.Copy, [("ps", pv_)], [("Vs", b)])
                            CP(stk[0:8, :], ps[pv_][0:8, 0:128], [("ps", pv_)], ["stk"])
                            dst = (owv_d if which == "v" else owk_d)[b, 2040:2048, a, :]
                            P.dma("sp", dst, stk[0:8, :], reads=["stk"], writes=[("onew", which, a, b)], sem="stk")
            if STOP == 10:
                P.frozen = True
            for g in range(8):
                kts = list(range(max(0, 4 * g - 16), 4 * g + 4))
                for kt in kts:
                    o0 = 4 * g - kt
                    sb_ = ci % 2
                    ci += 1
                    MM(ps[sb_][:, 0:512], KTa[:, kt * 128:(kt + 1) * 128], QTa[:, g * 512:(g + 1) * 512], True, True,
                       ["KTa", "QTa"], [("ps", sb_)])
                    TTo(tS[sb_], ps[sb_][:, 0:512], BT[:, (o0 + 3) * 128:(o0 + 7) * 128], ALU.add,
                        [("ps", sb_), "BT"], [("tS", sb_)])
                    ACT(PT[sb_], tS[sb_], AF.Exp, [("tS", sb_)], [("PT", sb_)])
                    for qb in range(4):
                        o = o0 + qb
                        if 0 <= o <= 16:
                            qi = 4 * g + qb
                            MM(ps[2 + qb][:, 0:129], PT[sb_][:, qb * 128:(qb + 1) * 128], Va[:, kt, 0:129],
                               kt == max(0, qi - 16), kt == qi, [("PT", sb_), "Va", "Va1"], [("ps", 2 + qb)])
                gens = []
                for qb in range(4):
                    qi = 4 * g + qb
                    col = (qi % 8) * 128
                    gens.append(att_norm(128, ps[2 + qb], catT3[:, fc, col:col + 128], SG3[:, fc, qi // 8:qi // 8 + 1], True, fc,
                                         ("ps", 2 + qb), qb))
                run_interleaved(gens)
            if STOP == 11:
                P.frozen = True
            for b in range(4):
                s_ = b % 2
                P.dma("pool", kcT[s_][:, 0:2048], ckT_d[b, a], writes=[("kcT", s_)], sem=("kcT", s_))
                P.dma("pool", vc[s_][:, :, 0:128], cvl_d[b, a].rearrange("p (t e) -> p t e", e=128),
                      writes=[("vc", s_)], sem=("vc", s_))
                qcols = QTa[:, T + 8 * b:T + 8 * b + 8]
                CP(kcT[s_][:, 2048:2056], KTa[:, T + 8 * b:T + 8 * b + 8], ["KTa"], [("kcT", s_)])
                for tl in range(16):
                    MM(ps[0][:, tl * 8:(tl + 1) * 8], kcT[s_][:, tl * 128:(tl + 1) * 128], qcols, True, True,
                       [("kcT", s_), "QTa"], [("ps", 0)])
                MM(ps[0][0:8, 128:136], kcT[s_][:, 2048:2056], qcols, True, True, [("kcT", s_), "QTa"], [("ps", 0)])
                TTo(tS[0][:, 0:128], ps[0][:, 0:128], BS[:, 0:128], ALU.add, [("ps", 0), "BS"], [("tS", 0)])
                TTo(tS[0][0:8, 128:136], ps[0][0:8, 128:136], BS[0:8, 128:136], ALU.add, [("ps", 0), "BS"], [("tS", 0)])
                ACT(PT[0][:, 0:128], tS[0][:, 0:128], AF.Exp, [("tS", 0)], [("PT", 0)])
                ACT(PT[0][0:8, 128:136], tS[0][0:8, 128:136], AF.Exp, [("tS", 0)], [("PT", 0)])
                for tl in range(16):
                    MM(ps[2 + b][0:8, 0:129], PT[0][:, tl * 8:(tl + 1) * 8], vc[s_][:, tl, 0:129], tl == 0, False,
                       [("PT", 0), ("vc", s_), ("vc1", s_)], [("ps", 2 + b)])
                MM(ps[2 + b][0:8, 0:129], PT[0][0:8, 128:136], Vs[0:8, b, 0:129], False, True,
                   [("PT", 0), ("Vs", b), "Vs1"], [("ps", 2 + b)])
            gens = []
            for b in range(4):
                col = 1024 + 8 * b
                gens.append(att_norm(8, ps[2 + b], catT3[:, fc, col:col + 8], gain[:, fc:fc + 1], False, fc, ("ps", 2 + b), b))
            run_interleaved(gens)
        P.barrier()
        if STOP == 2:
            P.frozen = True

        a3 = Al(P_END)
        Wo3 = v3(a3.b(KC * 512), 512)
        ln1g = a3.f(D)
        ln1b = a3.f(D)
        xo = [a3.f(512) for _ in range(2)]
        x1t = a3.f(D)
        x1Tf = a3.f(KC * 128)
        x1Tf3 = v3(x1Tf, 128)
        Wr3 = v3(a3.f(KC * 36), 36)
        lg = a3.f(36)
        gmax = a3.f(1)
        ngmax = a3.f(1)
        gm = a3.f(4)
        gex = a3.f(4)
        gsum = a3.f(1)
        gw = a3.f(1)
        pen = a3.f(4)
        em = a3.f(32)
        em2 = a3.f(32)
        mk1 = a3.f(32)
        mk2 = a3.f(32)
        m1 = a3.f(1)
        m2 = a3.f(1)
        dd_ = a3.f(1)
        w1 = a3.f(1)
        w1g = a3.f(1)
        w2g = a3.f(1)
        gt = a3.f(32)
        l_s1 = a3.f(1)
        l_s2 = a3.f(1)
        l_mean = a3.f(1)
        l_t1 = a3.f(1)
        l_nt = a3.f(1)
        l_sd = a3.f(1)
        l_rstd = a3.f(1)
        jk2 = a3.b(D)
        P.dma("sp", ln1g, lnp_d[0], writes=["ln1g"])
        P.dma("sp", ln1b, lnp_d[1], writes=["ln1b"])
        P.dma("sp", Wr3, wr_d.rearrange("(kc p) n -> p kc n", p=128), writes=["Wr"])
        woutv = wout_d.rearrange("(kc p) n -> p kc n", p=128)
        xi = 0
        for cg in range(4):
            P.dma("pool", Wo3, woutv[:, :, cg * 512:(cg + 1) * 512], writes=["Wo"], sem="Wo")
            for tt in range(9):
                Lr = 128 if tt < 8 else 32
                xs = xi % 2
                xi += 1
                P.dma("sp", xo[xs][0:Lr, :], xown_d[tt * 128:tt * 128 + Lr, cg * 512:(cg + 1) * 512],
                      writes=[("xo", xs)], sem=("xo", xs))
                pb = tt % 2
                for kc in range(KC):
                    MM(ps[pb][0:Lr, :], catT3[:, kc, tt * 128:tt * 128 + Lr], Wo3[:, kc, :], kc == 0, kc == KC - 1,
                       [("catT", kc), "Wo"], [("ps", pb)])
                STT(acc3[0:Lr, tt, cg * 512:(cg + 1) * 512], xo[xs][0:Lr, :], ALPHA, ps[pb][0:Lr, :], ALU.mult, ALU.add,
                    [("xo", xs), ("ps", pb)], [("acc", tt)])

        def layer_norm(Lr, src, srck, gtile, btile, gk, bk, dst, dstk):
            ACT(jk2[0:Lr, :], src, AF.Copy, [srck], ["jk2", "l_s1"], accum_out=l_s1[0:Lr, :])
            ACT(jk2[0:Lr, :], src, AF.Square, [srck], ["jk2", "l_s2"], accum_out=l_s2[0:Lr, :])
            TS(l_mean[0:Lr, :], l_s1[0:Lr, :], 1.0 / D, None, ALU.mult, None, ["l_s1"], ["l_mean"])
            TS(l_t1[0:Lr, :], l_s2[0:Lr, :], 1.0 / D, EPS, ALU.mult, ALU.add, ["l_s2"], ["l_t1"])
            STT(l_nt[0:Lr, :], l_mean[0:Lr, :], l_mean[0:Lr, :], l_t1[0:Lr, :], ALU.mult, ALU.subtract,
                ["l_mean", "l_t1"], ["l_nt"])
            ACT(l_sd[0:Lr, :], l_nt[0:Lr, :], AF.Sqrt, ["l_nt"], ["l_sd"], scale=-1.0)
            RCP(l_rstd[0:Lr, :], l_sd[0:Lr, :], ["l_sd"], ["l_rstd"])
            TS(dst, src, l_mean[0:Lr, :], l_rstd[0:Lr, :], ALU.subtract, ALU.mult, [srck, "l_mean", "l_rstd"], [dstk])
            TTo(dst, dst, gtile[0:Lr, :], ALU.mult, [dstk, gk], [dstk])
            TTo(dst, dst, btile[0:Lr, :], ALU.add, [dstk, bk], [dstk])

        for tt in range(9):
            Lr = 128 if tt < 8 else 32
            c0 = tt * 128
            layer_norm(Lr, acc3[0:Lr, tt, :], ("acc", tt), ln1g, ln1b, "ln1g", "ln1b", x1t[0:Lr, :], "x1t")
            ACT(acc3[0:Lr, tt, :], x1t[0:Lr, :], AF.Copy, ["x1t"], [("acc", tt)], scale=ALPHA)
            for k4 in range(4):
                pb = 2 + k4 % 2
                for q in range(4):
                    kc = k4 * 4 + q
                    TR(ps[pb][:, q * 128:q * 128 + Lr], x1t[0:Lr, kc * 128:(kc + 1) * 128], identF[0:Lr, 0:Lr],
                       ["x1t", "cst"], [("ps", pb)])
                src4 = ps[pb][:, :].rearrange("p (a b) -> p a b", b=128)[:, :, 0:Lr]
                CP(x1Tf3[:, k4 * 4:(k4 + 1) * 4, 0:Lr], src4, [("ps", pb)], ["x1Tf"])
                ACT(x1Tb3[:, k4 * 4:(k4 + 1) * 4, c0:c0 + Lr], src4, AF.Copy, [("ps", pb)], ["x1Tb"])
            for kc in range(KC):
                MM(ps[4][0:Lr, 0:36], x1Tf3[:, kc, 0:Lr], Wr3[:, kc, :], kc == 0, kc == KC - 1, ["x1Tf", "Wr"], [("ps", 4)])
            R = slice(0, Lr)
            TTo(lg[R, :], ps[4][R, 0:36], brt[R, :], ALU.add, [("ps", 4), "pvec"], ["lg"])
            RMAX(gmax[R, :], lg[R, 0:4], ["lg"], ["gmax"])
            TS(gm[R, :], lg[R, 0:4], gmax[R, :], None, ALU.is_equal, None, ["lg", "gmax"], ["gm"])
            TS(ngmax[R, :], gmax[R, :], -1.0, None, ALU.mult, None, ["gmax"], ["ngmax"])
            ACT(gex[R, :], lg[R, 0:4], AF.Exp, ["lg", "ngmax"], ["gex", "gsum"], bias=ngmax[R, :], accum_out=gsum[R, :])
            RCP(gw[R, :], gsum[R, :], ["gsum"], ["gw"])
            TS(pen[R, :], gm[R, :], -1.0, 1e30, ALU.add, ALU.mult, ["gm"], ["pen"])
            for g_ in range(4):
                TS(em[R, g_ * 8:(g_ + 1) * 8], lg[R, 4 + g_ * 8:12 + g_ * 8], pen[R, g_:g_ + 1], None, ALU.add, None,
                   ["lg", "pen"], ["em"])
            RMAX(m1[R, :], em[R, :], ["em"], ["m1"])
            TS(mk1[R, :], em[R, :], m1[R, :], None, ALU.is_equal, None, ["em", "m1"], ["mk1"])
            STT(em2[R, :], mk1[R, :], -1e30, em[R, :], ALU.mult, ALU.add, ["mk1", "em"], ["em2"])
            RMAX(m2[R, :], em2[R, :], ["em2"], ["m2"])
            TS(mk2[R, :], em2[R, :], m2[R, :], None, ALU.is_equal, None, ["em2", "m2"], ["mk2"])
            TTo(dd_[R, :], m1[R, :], m2[R, :], ALU.subtract, ["m1", "m2"], ["dd"])
            ACT(w1[R, :], dd_[R, :], AF.Sigmoid, ["dd"], ["w1"])
            TTo(w1g[R, :], w1[R, :], gw[R, :], ALU.mult, ["w1", "gw"], ["w1g"])
            TTo(w2g[R, :], gw[R, :], w1g[R, :], ALU.subtract, ["gw", "w1g"], ["w2g"])
            TS(gt[R, :], mk1[R, :], w1g[R, :], None, ALU.mult, None, ["mk1", "w1g"], ["gt"])
            STT(gates3[R, tt, :], mk2[R, :], w2g[R, :], gt[R, :], ALU.mult, ALU.add, ["mk2", "w2g", "gt"], ["gates"])
        assert a3.o <= ACC0, (a3.o, ACC0)
        P.barrier()
        if STOP == 3:
            P.frozen = True

        a4 = Al(CAT_OFF)
        WGs = [v3(a4.b(KC * 128), 128) for _ in range(3)]
        WUs = [v3(a4.b(KC * 128), 128) for _ in range(3)]
        WDs = [v3(a4.b(8 * 512), 512) for _ in range(3)]
        hT3 = v3(a4.b(8 * NO), NO)
        sg2 = [a4.b(352) for _ in range(2)]
        ln2g = a4.f(D)
        ln2b = a4.f(D)
        ost = a4.f(D)
        jk3 = a4.b(D)
        m_s1 = a4.f(1)
        P.dma("sp", ln2g, lnp_d[2], writes=["ln2g"])
        P.dma("sp", ln2b, lnp_d[3], writes=["ln2b"])
        gi = 0
        di = 0
        oi = 0
        for e in range(E_RUN):
            for ffc in range(8):
                sl = gi % 3
                gi += 1
                P.dma("pool", WGs[sl], wg_d[e, ffc].rearrange("p (kc n) -> p kc n", n=128), writes=[("WG", sl)], sem=("WG", sl))
                P.dma("pool", WUs[sl], wu_d[e, ffc].rearrange("p (kc n) -> p kc n", n=128), writes=[("WU", sl)], sem=("WU", sl))
                for tg in range(3):
                    pg, pu = (0, 1) if (ffc * 3 + tg) % 2 == 0 else (2, 3)
                    cols = slice(tg * 352, (tg + 1) * 352)
                    for kc in range(KC):
                        MM(ps[pg][:, 0:352], WGs[sl][:, kc, :], x1Tb3[:, kc, cols], kc == 0, kc == KC - 1,
                           [("WG", sl), "x1Tb"], [("ps", pg)])
                    for kc in range(KC):
                        MM(ps[pu][:, 0:352], WUs[sl][:, kc, :], x1Tb3[:, kc, cols], kc == 0, kc == KC - 1,
                           [("WU", sl), "x1Tb"], [("ps", pu)])
                    s2_ = (ffc * 3 + tg) % 2
                    ACT(sg2[s2_], ps[pg][:, 0:352], AF.Silu, [("ps", pg)], [("sg2", s2_)])
                    TTo(hT3[:, ffc, cols], sg2[s2_], ps[pu][:, 0:352], ALU.mult, [("sg2", s2_), ("ps", pu)], [("hT", ffc)])
            wdv = wd_d[e].rearrange("(fc p) n -> p fc n", p=128)
            for cg in range(4):
                sl = di % 3
                di += 1
                P.dma("pool", WDs[sl], wdv[:, :, cg * 512:(cg + 1) * 512], writes=[("WD", sl)], sem=("WD", sl))
                for tt in range(9):
                    Lr = 128 if tt < 8 else 32
                    po = 4 + oi % 4
                    oi += 1
                    for ffc in range(8):
                        MM(ps[po][0:Lr, :], hT3[:, ffc, tt * 128:tt * 128 + Lr], WDs[sl][:, ffc, :], ffc == 0, ffc == 7,
                           [("hT", ffc), ("WD", sl)], [("ps", po)])
                    dst = acc3[0:Lr, tt, cg * 512:(cg + 1) * 512]
                    STT(dst, ps[po][0:Lr, :], gates3[0:Lr, tt, e:e + 1], dst, ALU.mult, ALU.add,
                        [("ps", po), "gates", ("acc", tt, cg)], [("acc", tt, cg)])
        jk2 = jk3
        l_s1, l_s2, l_mean, l_t1, l_nt, l_sd, l_rstd = (a4.f(1) for _ in range(7))
        assert a4.o <= ACC0, (a4.o, ACC0)
        for tt in range(9):
            Lr = 128 if tt < 8 else 32
            srck = [("acc", tt, cg) for cg in range(4)]
            src = acc3[0:Lr, tt, :]
            ACT(jk2[0:Lr, :], src, AF.Copy, srck, ["jk2", "l_s1"], accum_out=l_s1[0:Lr, :])
            ACT(jk2[0:Lr, :], src, AF.Square, srck, ["jk2", "l_s2"], accum_out=l_s2[0:Lr, :])
            TS(l_mean[0:Lr, :], l_s1[0:Lr, :], 1.0 / D, None, ALU.mult, None, ["l_s1"], ["l_mean"])
            TS(l_t1[0:Lr, :], l_s2[0:Lr, :], 1.0 / D, EPS, ALU.mult, ALU.add, ["l_s2"], ["l_t1"])
            STT(l_nt[0:Lr, :], l_mean[0:Lr, :], l_mean[0:Lr, :], l_t1[0:Lr, :], ALU.mult, ALU.subtract,
                ["l_mean", "l_t1"], ["l_nt"])
            ACT(l_sd[0:Lr, :], l_nt[0:Lr, :], AF.Sqrt, ["l_nt"], ["l_sd"], scale=-1.0)
            RCP(l_rstd[0:Lr, :], l_sd[0:Lr, :], ["l_sd"], ["l_rstd"])
            TS(ost[0:Lr, :], src, l_mean[0:Lr, :], l_rstd[0:Lr, :], ALU.subtract, ALU.mult, srck + ["l_mean", "l_rstd"], ["ost"])
            TTo(ost[0:Lr, :], ost[0:Lr, :], ln2g[0:Lr, :], ALU.mult, ["ost", "ln2g"], ["ost"])
            TTo(ost[0:Lr, :], ost[0:Lr, :], ln2b[0:Lr, :], ALU.add, ["ost", "ln2b"], ["ost"])
            P.dma("sp", y_d[tt * 128:tt * 128 + Lr, :], ost[0:Lr, :], reads=["ost"], writes=[("y", tt)], sem="yo")
        P.build()
        for g in reversed(pctx):
            g.__exit__(None, None, None)
    return nc, P


_CACHE = {}


def _tables():
    slopes = np.array([2.0 ** (-8.0 * (h + 1) / 8) for h in range(8)], np.float64)

    def lbias(delta, a):
        cnt = ((delta >= 0) & (delta <= 128)).astype(np.float64)
        cnt += ((delta >= 0) & (delta % 4 == 0) & (delta <= 512))
        cnt += ((delta >= 0) & (delta % 16 == 0) & (delta <= 2048))
        out = np.full(delta.shape, NEG, np.float64)
        ok = cnt > 0
        out[ok] = -slopes[a] * delta[ok] + np.log(cnt[ok])
        return out

    bt = np.zeros((8, 128, 23, 128), np.float32)
    kr = np.arange(128)[:, None]
    qc = np.arange(128)[None, :]
    for a in range(8):
        for oi in range(23):
            o = oi - 3
            bt[a, :, oi, :] = lbias(128 * o + qc - kr, a)
    bs = np.full((8, 128, 136), NEG, np.float32)
    q8 = np.arange(8)[None, :]
    for a in range(8):
        for tl in range(16):
            bs[a, :, tl * 8:(tl + 1) * 8] = lbias(2048 + q8 - (tl * 128 + kr), a)
        bs[a, 0:8, 128:136] = lbias(q8 - np.arange(8)[:, None], a)
    cst = np.zeros((128, 512), np.float32)
    cst[:, 0:128] = np.eye(128, dtype=np.float32)
    cst[:, 128:256] = 1.0
    cst[0:64, 256:320] = np.triu(np.ones((64, 64), np.float32))
    return bt.reshape(8, 128, 23 * 128), bs, cst


def kernel(**inp):
    f = lambda k: np.asarray(inp[k], np.float32)
    xp, xsm = f("x_prompt"), f("x_sample")
    if "nc" not in _CACHE:
        _CACHE["nc"] = build_program()
    nc, P = _CACHE["nc"]
    bt, bs, cst = _tables()
    w_in = f("w_in")[0]
    w_in_l = np.ascontiguousarray(np.concatenate([w_in[:, 0:4096], w_in[:, 4104:7176]], axis=1)
                                  .reshape(KC, 128, 56, 128).transpose(2, 1, 0, 3)).reshape(56, 128, KC * 128)
    wgate8 = np.ascontiguousarray(w_in[:, 4096:4104])
    w_out = np.ascontiguousarray(f("w_out")[0])
    wg = np.ascontiguousarray(f("w_gate")[0].reshape(32, KC, 128, 8, 128).transpose(0, 3, 2, 1, 4)).reshape(32, 8, 128, KC * 128)
    wu = np.ascontiguousarray(f("w_up")[0].reshape(32, KC, 128, 8, 128).transpose(0, 3, 2, 1, 4)).reshape(32, 8, 128, KC * 128)
    wd = np.ascontiguousarray(f("w_down")[0])
    wr = np.ascontiguousarray(np.concatenate([f("w_group")[0], f("w_router")[0].transpose(1, 0, 2).reshape(D, 32)], axis=1))
    lnp = np.stack([np.broadcast_to(f(k)[0][None, :], (128, D)) for k in ("ln1_g", "ln1_b", "ln2_g", "ln2_b")]).astype(np.float32)
    conv_w, conv_b = f("conv_w")[0], f("conv_b")[0]
    gain = np.concatenate([f("mh_gain")[0], f("att_gain")[0]])
    brt = np.concatenate([f("b_group")[0], f("b_router")[0].reshape(32)])
    in_maps = []
    ncores = _CACHE.get("ncores", N_CORES)
    for c in range(ncores):
        b, s = c // 4, c % 4
        sb = slice(4 * c, 4 * c + 4)
        xs_flat = xsm[sb].reshape(32, D)
        pvec = np.zeros((128, 256), np.float32)
        pvec[:, 0:64] = conv_w.reshape(4, KC, 128).transpose(2, 1, 0).reshape(128, 64)
        pvec[:, 64:80] = conv_b.reshape(KC, 128).T
        pvec[:, 80:96] = gain.reshape(KC, 128).T
        pvec[:, 96 + s] = 1.0
        pvec[:, 100:108] = f("b_gate")[0][None, :]
        pvec[:, 108:144] = brt[None, :]
        sm = f("state_mlstm_m")[0, sb].reshape(16)
        pvec[:, 144:160] = sm[None, :]
        in_maps.append(dict(
            xTl=np.ascontiguousarray(xp[b].T.reshape(KC, 128, 8, 512).transpose(2, 1, 0, 3)).reshape(8, 128, KC * 512),
            xTs=np.ascontiguousarray(xs_flat.T.reshape(KC, 128, 32).transpose(1, 0, 2)).reshape(128, KC * 32),
            cvl=np.ascontiguousarray(f("cache_win_v")[0, sb].reshape(4, 16, 128, 8, 128).transpose(0, 3, 2, 1, 4)).reshape(4, 8, 128, 2048),
            xown=np.ascontiguousarray(np.concatenate([xp[b, 1024 * s:1024 * (s + 1)], xs_flat], axis=0)),
            w_in_l=w_in_l, wgate8=wgate8, w_out=w_out, pvec=pvec, srow=sm[None, :].copy(), lnp=lnp, wr=wr, wg=wg, wu=wu, wd=wd,
            sconvT=np.ascontiguousarray(f("state_conv")[0, sb].transpose(0, 2, 1)),
            sC=np.ascontiguousarray(f("state_mlstm_C")[0, sb]), sn=np.ascontiguousarray(f("state_mlstm_n")[0, sb]),
            ckT=np.ascontiguousarray(f("cache_win_k")[0, sb].transpose(0, 2, 3, 1)),
            ck=np.ascontiguousarray(f("cache_win_k")[0, sb]), cv=np.ascontiguousarray(f("cache_win_v")[0, sb]),
            cst=cst, bt=bt, bs=bs))
    res = run_bass_kernel_spmd(nc, in_maps, core_ids=list(range(ncores))).results
    _CACHE["res"] = res
    if ncores < N_CORES:
        return res
    y_p = np.zeros((2, T, D), np.float32)
    y_s = np.zeros((32, 8, D), np.float32)
    for c in range(8):
        b, s = c // 4, c % 4
        y_p[b, 1024 * s:1024 * (s + 1)] = res[c]["y"][0:1024]
        y_s[4 * c:4 * c + 4] = res[c]["y"][1024:1056].reshape(4, 8, D)
    lead = [res[0], res[4]]
    p_conv = np.stack([r["pconvT"].T for r in lead])[None]
    p_C = np.stack([r["pC"] for r in lead])[None]
    p_n = np.stack([r["pn"] for r in lead])[None]
    p_m = np.stack([r["pm"][0] for r in lead])[None]
    p_wk = np.stack([r["pwkT"].transpose(2, 0, 1) for r in lead])[None]
    p_wv = np.stack([r["pwv"] for r in lead])[None]
    s_conv = np.concatenate([r["oconvT"].transpose(0, 2, 1) for r in res])[None]
    s_C = np.concatenate([r["oC"] for r in res])[None]
    s_n = np.concatenate([r["on"] for r in res])[None]
    s_m = np.concatenate([r["om"].reshape(4, 4) for r in res])[None]
    s_wk = np.concatenate([r["owk"] for r in res])[None]
    s_wv = np.concatenate([r["owv"] for r in res])[None]
    outs = (y_p, y_s, p_conv, p_C, p_n, p_m, p_wk, p_wv, s_conv, s_C, s_n, s_m, s_wk, s_wv)
    return tuple(np.ascontiguousarray(o, dtype=np.float32) for o in outs)
```
